# Optimizing a Trainium2 kernel written in Bass

```python
import math
import jax, jax.numpy as jnp
from jax import lax
import numpy as np

D_MODEL = 2048
BATCH = 4
SEQ = 2048
DEPTH = 4
DEC_BATCH = 128
DEC_SEQ = 8
PAST_LEN = 16384
PAGE_SIZE = 128

D_SSM = D_MODEL // 2
SSM_GROUP = 16
N_SSM_GROUPS = D_SSM // SSM_GROUP
SSM_STATE = 64
D_CONV = D_MODEL // 2
CONV_WIDTH = 31
D_IN_PROJ = D_SSM + 2 * D_CONV + 2 * D_MODEL
N_EXPERT_GROUPS = 4
EXPERTS_PER_GROUP = 8
N_EXPERTS = N_EXPERT_GROUPS * EXPERTS_PER_GROUP
TOP_K_IN_GROUP = 2
D_EXPERT = D_MODEL // 8
ALPHA = (2 * DEPTH) ** 0.25
BETA = (8 * DEPTH) ** -0.25
LN_EPS = 1e-5
ADA_CHUNKS = 6

kernel_name = "gated_s5_conformer_hmoe_decoder_step"


def _layer_norm(x, g=None, b=None):
    xf = x.astype(jnp.float32)
    mu = jnp.mean(xf, axis=-1, keepdims=True)
    var = jnp.mean(jnp.square(xf - mu), axis=-1, keepdims=True)
    y = (xf - mu) * lax.rsqrt(var + LN_EPS)
    if g is not None:
        y = y * g.astype(jnp.float32) + b.astype(jnp.float32)
    return y.astype(x.dtype)


def _complex_affine_combine(e1, e2):
    ar1, ai1, br1, bi1 = e1
    ar2, ai2, br2, bi2 = e2
    return (ar1 * ar2 - ai1 * ai2,
            ar1 * ai2 + ai1 * ar2,
            ar2 * br1 - ai2 * bi1 + br2,
            ar2 * bi1 + ai2 * br1 + bi2)


def _s5_scan(u, h0_re, h0_im, a_re, a_im, log_dt, b_re, b_im, c_re, c_im, d_skip):
    f32 = jnp.float32
    bsz, seq, _ = u.shape
    uf = u.astype(f32).reshape(bsz, seq, N_SSM_GROUPS, SSM_GROUP)
    a_re = a_re.astype(f32)
    a_im = a_im.astype(f32)
    dt = jnp.exp(log_dt.astype(f32))[:, None]
    mag = jnp.exp(dt * a_re)
    ang = dt * a_im
    ab_re = mag * jnp.cos(ang)
    ab_im = mag * jnp.sin(ang)
    den = jnp.square(a_re) + jnp.square(a_im)
    k_re = ((ab_re - 1.0) * a_re + ab_im * a_im) / den
    k_im = (ab_im * a_re - (ab_re - 1.0) * a_im) / den
    b_re = b_re.astype(f32)
    b_im = b_im.astype(f32)
    bb_re = k_re[..., None] * b_re - k_im[..., None] * b_im
    bb_im = k_re[..., None] * b_im + k_im[..., None] * b_re
    bu_re = jnp.einsum('bsgk,gpk->bsgp', uf, bb_re)
    bu_im = jnp.einsum('bsgk,gpk->bsgp', uf, bb_im)
    h0_re = h0_re.astype(f32)
    h0_im = h0_im.astype(f32)
    bu_re = bu_re.at[:, 0].add(ab_re * h0_re - ab_im * h0_im)
    bu_im = bu_im.at[:, 0].add(ab_re * h0_im + ab_im * h0_re)
    ar = jnp.broadcast_to(ab_re, bu_re.shape)
    ai = jnp.broadcast_to(ab_im, bu_re.shape)
    _, _, s_re, s_im = lax.associative_scan(_complex_affine_combine, (ar, ai, bu_re, bu_im), axis=1)
    y = (jnp.einsum('bsgp,gkp->bsgk', s_re, c_re.astype(f32))
         - jnp.einsum('bsgp,gkp->bsgk', s_im, c_im.astype(f32))
         + d_skip.astype(f32).reshape(N_SSM_GROUPS, SSM_GROUP) * uf)
    return (y.reshape(bsz, seq, D_SSM).astype(u.dtype),
            s_re[:, -1].astype(u.dtype), s_im[:, -1].astype(u.dtype))


def _causal_depthwise_conv(u, buf, w, b):
    padded = jnp.concatenate([buf.astype(u.dtype), u], axis=1)
    y = lax.conv_general_dilated(padded, w[:, None, :].astype(u.dtype), window_strides=(1,),
                                 padding='VALID', dimension_numbers=('NWC', 'WIO', 'NWC'),
                                 feature_group_count=D_CONV)
    return y + b, padded[:, -(CONV_WIDTH - 1):]


def _hier_moe(h, w_group, b_group, w_router, b_router, w_up, w_gate, w_down):
    f32 = jnp.float32
    bsz, seq, d = h.shape
    hf = h.reshape(bsz * seq, d)
    glog = (hf @ w_group).astype(f32) + b_group.astype(f32)
    gprob = jax.nn.softmax(glog, axis=-1)
    gsel = jnp.argmax(glog, axis=-1)
    p_g = jnp.take_along_axis(gprob, gsel[:, None], axis=1)
    elog = ((hf @ w_router).astype(f32) + b_router.astype(f32)).reshape(-1, N_EXPERT_GROUPS, EXPERTS_PER_GROUP)
    sel_idx = jnp.broadcast_to(gsel[:, None, None], (elog.shape[0], 1, EXPERTS_PER_GROUP))
    elog_sel = jnp.take_along_axis(elog, sel_idx, axis=1)[:, 0]
    topv, topi = lax.top_k(elog_sel, TOP_K_IN_GROUP)
    wts = jax.nn.softmax(topv, axis=-1) * p_g
    expert_idx = gsel[:, None] * EXPERTS_PER_GROUP + topi
    combine = jnp.sum(jax.nn.one_hot(expert_idx, N_EXPERTS, dtype=f32) * wts[..., None], axis=1)
    up = jnp.einsum('td,edf->tef', hf, w_up)
    gt = jnp.einsum('td,edf->tef', hf, w_gate)
    act = jax.nn.silu(gt) * up * combine.astype(h.dtype)[..., None]
    y = jnp.einsum('tef,efd->td', act, w_down)
    return y.reshape(bsz, seq, d)


def _layer(x, c, h0_re, h0_im, conv_buf,
           w_ada, b_ada, w_in, ssm_a_re, ssm_a_im, ssm_log_dt, ssm_b_re, ssm_b_im, ssm_c_re, ssm_c_im,
           ssm_d, w_s5_val, w_s5_gate, conv_w, conv_b, conv_ln_g, conv_ln_b, w_conv_pw, w_out,
           ln1_g, ln1_b, moe_w_group, moe_b_group, moe_w_router, moe_b_router,
           moe_w_up, moe_w_gate, moe_w_down, ln2_g, ln2_b):
    ada = jax.nn.silu(c) @ w_ada + b_ada
    sh1, sc1, g1, sh2, sc2, g2 = jnp.split(ada[:, None, :], ADA_CHUNKS, axis=-1)

    h = _layer_norm(x) * (1.0 + sc1) + sh1
    proj = h @ w_in
    u_s = proj[..., :D_SSM]
    u_c2 = proj[..., D_SSM:D_SSM + 2 * D_CONV]
    gates = jax.nn.sigmoid(proj[..., D_SSM + 2 * D_CONV:])
    g_s, g_c = gates[..., :D_MODEL], gates[..., D_MODEL:]
    y_s, hT_re, hT_im = _s5_scan(u_s, h0_re, h0_im, ssm_a_re, ssm_a_im, ssm_log_dt,
                                 ssm_b_re, ssm_b_im, ssm_c_re, ssm_c_im, ssm_d)
    y_s = jax.nn.gelu(y_s)
    br_s = (y_s @ w_s5_val) * jax.nn.sigmoid(y_s @ w_s5_gate)
    u_c = u_c2[..., :D_CONV] * jax.nn.sigmoid(u_c2[..., D_CONV:])
    v, new_buf = _causal_depthwise_conv(u_c, conv_buf, conv_w, conv_b)
    v = jax.nn.silu(_layer_norm(v, conv_ln_g, conv_ln_b))
    br_c = v @ w_conv_pw
    mix = (g_s * br_s + g_c * br_c) @ w_out
    x = _layer_norm(ALPHA * x + g1 * mix, ln1_g, ln1_b)

    h = _layer_norm(x) * (1.0 + sc2) + sh2
    ff = _hier_moe(h, moe_w_group, moe_b_group, moe_w_router, moe_b_router, moe_w_up, moe_w_gate, moe_w_down)
    x = _layer_norm(ALPHA * x + g2 * ff, ln2_g, ln2_b)
    return x, hT_re, hT_im, new_buf


def setup_inputs(seed: int = 0) -> dict:
    key = jax.random.key(seed)
    ks = iter(jax.random.split(key, 48))
    f32 = jnp.float32

    def nrm(shape, scale):
        return jax.random.normal(next(ks), shape, f32) * scale

    n_idx = jnp.arange(SSM_STATE, dtype=f32)
    inp = {}
    inp['x_prompt'] = nrm((BATCH, SEQ, D_MODEL), 1.0)
    inp['x_sample'] = nrm((DEC_BATCH, DEC_SEQ, D_MODEL), 1.0)
    inp['c_prompt'] = nrm((BATCH, D_MODEL), 1.0)
    inp['c_sample'] = nrm((DEC_BATCH, D_MODEL), 1.0)
    inp['state_ssm_re'] = nrm((DEPTH, DEC_BATCH, N_SSM_GROUPS, SSM_STATE), 0.3)
    inp['state_ssm_im'] = nrm((DEPTH, DEC_BATCH, N_SSM_GROUPS, SSM_STATE), 0.3)
    inp['state_conv'] = nrm((DEPTH, DEC_BATCH, CONV_WIDTH - 1, D_CONV), 0.5)
    inp['w_ada'] = nrm((DEPTH, D_MODEL, ADA_CHUNKS * D_MODEL), 0.5 * D_MODEL ** -0.5)
    inp['b_ada'] = nrm((DEPTH, ADA_CHUNKS * D_MODEL), 0.02)
    inp['w_in'] = nrm((DEPTH, D_MODEL, D_IN_PROJ), D_MODEL ** -0.5)
    inp['ssm_a_re'] = -0.5 + nrm((DEPTH, N_SSM_GROUPS, SSM_STATE), 0.01)
    inp['ssm_a_im'] = math.pi * n_idx + nrm((DEPTH, N_SSM_GROUPS, SSM_STATE), 0.01)
    inp['ssm_log_dt'] = jax.random.uniform(next(ks), (DEPTH, N_SSM_GROUPS), f32,
                                           minval=math.log(1e-3), maxval=math.log(1e-1))
    inp['ssm_b_re'] = nrm((DEPTH, N_SSM_GROUPS, SSM_STATE, SSM_GROUP), (2 * SSM_GROUP) ** -0.5)
    inp['ssm_b_im'] = nrm((DEPTH, N_SSM_GROUPS, SSM_STATE, SSM_GROUP), (2 * SSM_GROUP) ** -0.5)
    inp['ssm_c_re'] = nrm((DEPTH, N_SSM_GROUPS, SSM_GROUP, SSM_STATE), (2 * SSM_STATE) ** -0.5)
    inp['ssm_c_im'] = nrm((DEPTH, N_SSM_GROUPS, SSM_GROUP, SSM_STATE), (2 * SSM_STATE) ** -0.5)
    inp['ssm_d'] = nrm((DEPTH, D_SSM), 1.0)
    inp['w_s5_val'] = nrm((DEPTH, D_SSM, D_MODEL), D_SSM ** -0.5)
    inp['w_s5_gate'] = nrm((DEPTH, D_SSM, D_MODEL), D_SSM ** -0.5)
    inp['conv_w'] = nrm((DEPTH, CONV_WIDTH, D_CONV), CONV_WIDTH ** -0.5)
    inp['conv_b'] = nrm((DEPTH, D_CONV), 0.02)
    inp['conv_ln_g'] = 1.0 + nrm((DEPTH, D_CONV), 0.02)
    inp['conv_ln_b'] = nrm((DEPTH, D_CONV), 0.02)
    inp['w_conv_pw'] = nrm((DEPTH, D_CONV, D_MODEL), D_CONV ** -0.5)
    inp['w_out'] = nrm((DEPTH, D_MODEL, D_MODEL), BETA * D_MODEL ** -0.5)
    inp['ln1_g'] = 1.0 + nrm((DEPTH, D_MODEL), 0.02)
    inp['ln1_b'] = nrm((DEPTH, D_MODEL), 0.02)
    inp['moe_w_group'] = nrm((DEPTH, D_MODEL, N_EXPERT_GROUPS), D_MODEL ** -0.5)
    inp['moe_b_group'] = nrm((DEPTH, N_EXPERT_GROUPS), 0.01)
    inp['moe_w_router'] = nrm((DEPTH, D_MODEL, N_EXPERTS), D_MODEL ** -0.5)
    inp['moe_b_router'] = nrm((DEPTH, N_EXPERTS), 0.01)
    inp['moe_w_up'] = nrm((DEPTH, N_EXPERTS, D_MODEL, D_EXPERT), D_MODEL ** -0.5)
    inp['moe_w_gate'] = nrm((DEPTH, N_EXPERTS, D_MODEL, D_EXPERT), D_MODEL ** -0.5)
    inp['moe_w_down'] = nrm((DEPTH, N_EXPERTS, D_EXPERT, D_MODEL), BETA * D_EXPERT ** -0.5)
    inp['ln2_g'] = 1.0 + nrm((DEPTH, D_MODEL), 0.02)
    inp['ln2_b'] = nrm((DEPTH, D_MODEL), 0.02)
    return inp


def reference(x_prompt, x_sample, c_prompt, c_sample, state_ssm_re, state_ssm_im, state_conv,
              w_ada, b_ada, w_in, ssm_a_re, ssm_a_im, ssm_log_dt, ssm_b_re, ssm_b_im, ssm_c_re, ssm_c_im,
              ssm_d, w_s5_val, w_s5_gate, conv_w, conv_b, conv_ln_g, conv_ln_b, w_conv_pw, w_out,
              ln1_g, ln1_b, moe_w_group, moe_b_group, moe_w_router, moe_b_router,
              moe_w_up, moe_w_gate, moe_w_down, ln2_g, ln2_b):
    weights = (w_ada, b_ada, w_in, ssm_a_re, ssm_a_im, ssm_log_dt, ssm_b_re, ssm_b_im, ssm_c_re, ssm_c_im,
               ssm_d, w_s5_val, w_s5_gate, conv_w, conv_b, conv_ln_g, conv_ln_b, w_conv_pw, w_out,
               ln1_g, ln1_b, moe_w_group, moe_b_group, moe_w_router, moe_b_router,
               moe_w_up, moe_w_gate, moe_w_down, ln2_g, ln2_b)
    n_prompt = x_prompt.shape[0]
    zero_ssm = jnp.zeros((n_prompt, N_SSM_GROUPS, SSM_STATE), x_prompt.dtype)
    zero_conv = jnp.zeros((n_prompt, CONV_WIDTH - 1, D_CONV), x_prompt.dtype)

    xp, xs = x_prompt, x_sample
    p_re, p_im, p_conv, s_re, s_im, s_conv = [], [], [], [], [], []
    for l in range(DEPTH):
        lw = tuple(w[l] for w in weights)
        xp, hr, hi, cb = _layer(xp, c_prompt, zero_ssm, zero_ssm, zero_conv, *lw)
        p_re.append(hr); p_im.append(hi); p_conv.append(cb)
        xs, hr, hi, cb = _layer(xs, c_sample, state_ssm_re[l], state_ssm_im[l], state_conv[l], *lw)
        s_re.append(hr); s_im.append(hi); s_conv.append(cb)

    new_ssm_re_prompt = jnp.stack(p_re, axis=0)
    new_ssm_im_prompt = jnp.stack(p_im, axis=0)
    new_conv_prompt = jnp.stack(p_conv, axis=0)
    new_ssm_re_sample = jnp.stack(s_re, axis=0)
    new_ssm_im_sample = jnp.stack(s_im, axis=0)
    new_conv_sample = jnp.stack(s_conv, axis=0)
    return (xp, xs, new_ssm_re_prompt, new_ssm_im_prompt, new_conv_prompt,
            new_ssm_re_sample, new_ssm_im_sample, new_conv_sample)
```

```python
import math
import numpy as np
from contextlib import ExitStack
import concourse.bass as bass
import concourse.mybir as mybir
from concourse.bass_utils import run_bass_kernel_spmd

F32 = mybir.dt.float32
BF16 = mybir.dt.bfloat16
AF = mybir.ActivationFunctionType
ALU = mybir.AluOpType

D = 2048; DEPTH = 4; NT = 1152; NP = 1024; KQ = 16
ALPHA = (2 * DEPTH) ** 0.25
LN_EPS = 1e-5
NCORES = 8
ARENA = 53200
BLK = 256


def _ap_keys(ap):
    sp = str(ap.space).upper()
    if not ("SB" in sp or "PSUM" in sp):
        return None
    es = 2 if "bfloat16" in str(ap.dtype) else 4
    dims = list(ap.ap)
    pstep = dims[0][0]
    off = ap.offset % pstep if pstep > 0 else ap.offset
    ext = 1
    for (st, cnt) in dims[1:]:
        ext += abs(st) * (cnt - 1)
    lo = off * es; hi = (off + ext) * es
    name = ap.tensor.name
    return [(name, b) for b in range(lo // BLK, (hi - 1) // BLK + 1)]


class Prog:
    COMPUTE = ("pe", "act", "dve", "pool")

    def __init__(self, nc, es, n_dma_sems=28, sem_cap=8000):
        self.nc = nc; self.es = es; self.ops = []
        self.eng = {"pe": nc.tensor, "act": nc.scalar, "dve": nc.vector, "pool": nc.gpsimd, "sp": nc.sync}
        self.n_dma_sems = n_dma_sems; self.sem_cap = sem_cap

    def op(self, eng, fn, ins=(), outs=(), r=(), w=(), dma=False):
        if getattr(self, "dry", False):
            return
        rk = list(r); wk = list(w)
        for a in ins:
            k = _ap_keys(a)
            if k: rk += k
        for a in outs:
            k = _ap_keys(a)
            if k: wk += k
        self.ops.append(dict(eng=eng, fn=fn, r=rk, w=wk, dma=dma, deps=None, need=False))

    def emit(self):
        nc, es, ops = self.nc, self.es, self.ops
        lastw = {}; readers = {}
        for i, o in enumerate(ops):
            deps = set()
            for k in o["r"]:
                d = lastw.get(k)
                if d is not None: deps.add(d)
            for k in o["w"]:
                d = lastw.get(k)
                if d is not None: deps.add(d)
                rd = readers.get(k)
                if rd:
                    deps.update(rd.values())
            deps.discard(i)
            if o["eng"] == "pe" and not o["dma"]:
                deps = {d for d in deps if not (ops[d]["eng"] == "pe" and not ops[d]["dma"])}
            o["deps"] = deps
            for d in deps: ops[d]["need"] = True
            ek = ("dma", i) if o["dma"] else o["eng"]
            for k in o["r"]:
                rd = readers.get(k)
                if rd is None: readers[k] = {ek: i}
                else: rd[ek] = i
            for k in o["w"]:
                lastw[k] = i; readers[k] = None
            o["r"] = None; o["w"] = None
        cnt = {e: 0 for e in self.COMPUTE}; epoch = {e: 0 for e in self.COMPUTE}
        sems = {e: [es.enter_context(nc.semaphore(f"s_{e}_0"))] for e in self.COMPUTE}
        dsems = [es.enter_context(nc.semaphore(f"s_dma_{i}")) for i in range(self.n_dma_sems)]
        dval = [0] * self.n_dma_sems; dnext = 0
        for o in ops:
            if o["dma"]:
                o["dsem"] = dnext; dnext = (dnext + 1) % self.n_dma_sems
            elif o["need"]:
                e = o["eng"]
                if cnt[e] >= self.sem_cap:
                    epoch[e] += 1; cnt[e] = 0
                    sems[e].append(es.enter_context(nc.semaphore(f"s_{e}_{epoch[e]}")))
                cnt[e] += 1
                o["sem"] = (e, epoch[e], cnt[e])
        waited = {}
        for o in ops:
            weng = o["eng"]
            for d in sorted(o["deps"]):
                od = ops[d]
                if od["dma"]:
                    key = (weng, ("d", od["dsem"]))
                    if waited.get(key, -1) >= od["dval"]: continue
                    waited[key] = od["dval"]
                    self.eng[weng].wait_ge(dsems[od["dsem"]], od["dval"])
                else:
                    e, ep, v = od["sem"]
                    key = (weng, ("c", e))
                    if waited.get(key, (-1, -1)) >= (ep, v): continue
                    waited[key] = (ep, v)
                    self.eng[weng].wait_ge(sems[e][ep], v)
            if o["dma"]:
                si = o["dsem"]
                if dval[si] > 0:
                    key = (weng, ("d", si))
                    if waited.get(key, -1) < dval[si]:
                        waited[key] = dval[si]
                        self.eng[weng].wait_ge(dsems[si], dval[si])
                ins = o["fn"]()
                dval[si] += 16; o["dval"] = dval[si]
                ins.then_inc(dsems[si], 16)
            else:
                ins = o["fn"]()
                if o["need"]:
                    e, ep, v = o["sem"]
                    ins.then_inc(sems[e][ep], 1)
            o["fn"] = None
        for si in range(self.n_dma_sems):
            if dval[si] > 0:
                nc.sync.wait_ge(dsems[si], dval[si])
        self.stats = dict(n_ops=len(ops), incs={e: (epoch[e], cnt[e]) for e in self.COMPUTE})


def build_program(depth=DEPTH, passes=("A", "B"), debug=None, stop_after=None):
    nc = bass.Bass("TRN2", target_bir_lowering=False)
    es = ExitStack()
    P = Prog(nc, es)

    def din(name, shape):
        return nc.dram_tensor(name, list(shape), F32, kind="ExternalInput").ap()

    def dout(name, shape):
        return nc.dram_tensor(name, list(shape), F32, kind="ExternalOutput").ap()

    def dscr(name, shape, dt=F32):
        return nc.dram_tensor(name, list(shape), dt, kind="Internal").ap()

    xp = din("xp", [2 * NP, D]); xs = din("xs", [16, 8, D]); cvec = din("cvec", [17, D])
    sre_i = din("sre", [DEPTH, 16, 64, 64]); sim_i = din("sim", [DEPTH, 16, 64, 64])
    scv_i = din("scv", [DEPTH, 16, 30, 1024])
    ident_d = din("ident", [128, 128]); shm_d = din("shm", [8, 128, 128])
    w_ada = din("w_ada", [DEPTH, D, 6 * D]); b_ada = din("b_ada", [DEPTH, 6 * D])
    w_in = din("w_in", [DEPTH, D, 7168])
    a_re_d = din("ssm_a_re", [DEPTH, 64, 64]); a_im_d = din("ssm_a_im", [DEPTH, 64, 64])
    ldt_d = din("ssm_log_dt", [DEPTH, 64])
    b_re_d = din("ssm_b_re", [DEPTH, 64, 64, 16]); b_im_d = din("ssm_b_im", [DEPTH, 64, 64, 16])
    c_re_d = din("ssm_c_re", [DEPTH, 64, 16, 64]); c_im_d = din("ssm_c_im", [DEPTH, 64, 16, 64])
    ssm_d_d = din("ssm_d", [DEPTH, 1024])
    w_val = din("w_s5_val", [DEPTH, 1024, D]); w_gate = din("w_s5_gate", [DEPTH, 1024, D])
    conv_w_d = din("conv_w", [DEPTH, 31, 1024]); conv_b_d = din("conv_b", [DEPTH, 1024])
    cln_g_d = din("conv_ln_g", [DEPTH, 1024]); cln_b_d = din("conv_ln_b", [DEPTH, 1024])
    w_pw = din("w_conv_pw", [DEPTH, 1024, D]); w_out = din("w_out", [DEPTH, D, D])
    ln1_g_d = din("ln1_g", [DEPTH, D]); ln1_b_d = din("ln1_b", [DEPTH, D])
    mwg_d = din("moe_w_group", [DEPTH, D, 4]); mbg_d = din("moe_b_group", [DEPTH, 4])
    mwr_d = din("moe_w_router", [DEPTH, D, 32]); mbr_d = din("moe_b_router", [DEPTH, 32])
    w_up = din("moe_w_up", [DEPTH, 32, D, 256]); w_gt = din("moe_w_gate", [DEPTH, 32, D, 256])
    w_dn = din("moe_w_down", [DEPTH, 32, 256, D])
    ln2_g_d = din("ln2_g", [DEPTH, D]); ln2_b_d = din("ln2_b", [DEPTH, D])

    yp_o = dout("yp", [2 * NP, D]); ys_o = dout("ys", [16, 8, D])
    pre_o = dout("pre", [DEPTH, 64, 64]); pim_o = dout("pim", [DEPTH, 64, 64])
    pcv_o = dout("pcv", [DEPTH, 30, 1024])
    sre_o = dout("sre_o", [DEPTH, 16, 64, 64]); sim_o = dout("sim_o", [DEPTH, 16, 64, 64])
    scv_o = dout("scv_o", [DEPTH, 16, 30, 1024])
    dbg_o = {}
    if debug:
        for k, (shp, dtn) in debug.items():
            dbg_o[k] = nc.dram_tensor("dbg_" + k, list(shp), BF16 if dtn == "bf16" else F32, kind="ExternalOutput").ap()

    xscr = dscr("xscr", [KQ, 128, NT])
    ada_scr = dscr("ada_scr", [DEPTH, 128, 96 * 17])
    R_scr = dscr("R_scr", [DEPTH, 128, 8192], BF16)
    T_scr = dscr("T_scr", [DEPTH, 128, 8192], BF16)
    O_scr = dscr("O_scr", [DEPTH, 2, 128, 4096])
    A8_scr = dscr("A8_scr", [DEPTH, 128, 64])

    arena = es.enter_context(nc.sbuf_tensor("arena", [128, ARENA], F32))
    cur = [0]

    def alloc(nwords):
        if nwords >= 256:
            cur[0] = (cur[0] + 63) // 64 * 64
        o = cur[0]; cur[0] += nwords
        assert cur[0] <= ARENA, ("SBUF arena overflow", cur[0], ARENA)
        return o

    def f32v(off, n):
        return arena[:, off:off + n]

    def bf16v(off, nbf):
        assert nbf % 2 == 0
        return arena[:, off:off + nbf // 2].bitcast(BF16)

    ps = [es.enter_context(nc.psum_tensor(f"ps{i}", [128, 512], F32)) for i in range(8)]
    psn = [0]

    def nb():
        i = psn[0] % 8; psn[0] += 1
        return ps[i]

    def MM(out, lhsT, rhs, start, stop):
        P.op("pe", lambda: nc.tensor.matmul(out, lhsT=lhsT, rhs=rhs, start=start, stop=stop), ins=[lhsT, rhs], outs=[out])

    def TR(out, in_, idn):
        P.op("pe", lambda: nc.tensor.transpose(out, in_, idn), ins=[in_, idn], outs=[out])

    def ACT(out, in_, func, scale=1.0, bias=0.0):
        ins = [in_] + [a for a in (scale, bias) if not isinstance(a, (int, float))]
        P.op("act", lambda: nc.scalar.activation(out=out, in_=in_, func=func, scale=scale, bias=bias), ins=ins, outs=[out])

    def TT(out, in0, in1, op):
        P.op("dve", lambda: nc.vector.tensor_tensor(out=out, in0=in0, in1=in1, op=op), ins=[in0, in1], outs=[out])

    def TS(out, in0, s1, s2, op0, op1=None):
        ins = [in0] + [a for a in (s1, s2) if a is not None and not isinstance(a, (int, float))]
        if s2 is None:
            P.op("dve", lambda: nc.vector.tensor_scalar(out=out, in0=in0, scalar1=s1, scalar2=None, op0=op0), ins=ins, outs=[out])
        else:
            P.op("dve", lambda: nc.vector.tensor_scalar(out=out, in0=in0, scalar1=s1, scalar2=s2, op0=op0, op1=op1), ins=ins, outs=[out])

    def STT(out, in0, scalar, in1, op0, op1):
        ins = [in0, in1] + ([] if isinstance(scalar, (int, float)) else [scalar])
        P.op("dve", lambda: nc.vector.scalar_tensor_tensor(out=out, in0=in0, scalar=scalar, in1=in1, op0=op0, op1=op1), ins=ins, outs=[out])

    def TTP(out, in0, in1, op):
        P.op("pool", lambda: nc.gpsimd.tensor_tensor(out=out, in0=in0, in1=in1, op=op), ins=[in0, in1], outs=[out])

    def TSP(out, in0, s1):
        P.op("pool", lambda: nc.gpsimd.tensor_scalar(out=out, in0=in0, scalar1=s1, scalar2=None, op0=ALU.mult), ins=[in0, s1], outs=[out])

    def ACP(out, in_):
        P.op("act", lambda: nc.scalar.activation(out=out, in_=in_, func=AF.Copy), ins=[in_], outs=[out])

    def CP(out, in_):
        P.op("dve", lambda: nc.vector.tensor_copy(out=out, in_=in_), ins=[in_], outs=[out])

    def MS(ap, val):
        P.op("dve", lambda: nc.vector.memset(ap, val), outs=[ap])

    def RCP(out, in_):
        P.op("dve", lambda: nc.vector.reciprocal(out=out, in_=in_), ins=[in_], outs=[out])

    def DMA(out, in_, r=(), w=(), q="sp"):
        e = {"sp": nc.sync, "pool": nc.gpsimd, "act": nc.scalar}[q]
        P.op(q, lambda: e.dma_start(out=out, in_=in_), ins=[in_], outs=[out], r=r, w=w, dma=True)

    ident = f32v(alloc(128), 128)
    identb = bf16v(alloc(64), 128)
    onesb = bf16v(alloc(64), 128)
    shm = f32v(alloc(1024), 1024).rearrange("p (i m) -> p i m", i=8)
    scT = bf16v(alloc(KQ * 18 // 2), KQ * 18).rearrange("p (k b) -> p k b", k=KQ)[:, :, 0:17]
    adaT = f32v(alloc(96 * 17), 96 * 17).rearrange("p (c b) -> p c b", c=96)
    pv1 = f32v(alloc(128), 128)
    pv2 = f32v(alloc(64), 64)
    cw = f32v(alloc(248), 248)
    gA = f32v(alloc(64), 64)
    rbias = f32v(alloc(36), 36)
    badaN = f32v(alloc(96), 96)
    astage = [f32v(alloc(68), 68) for _ in range(2)]
    A8 = f32v(alloc(64), 64)
    stsave = f32v(alloc(DEPTH * 64), DEPTH * 64).rearrange("p (l r g) -> p l r g", l=DEPTH, r=2)
    cvsave = f32v(alloc(DEPTH * 240), DEPTH * 240).rearrange("p (l q t) -> p l q t", l=DEPTH, q=8)
    hT = bf16v(alloc(KQ * NT // 2), KQ * NT).rearrange("p (k n) -> p k n", k=KQ)
    NWB = 3
    wb = [bf16v(alloc(4096), 8192) for _ in range(NWB)]
    wb3 = [w_.rearrange("p (k n) -> p k n", k=KQ) for w_ in wb]
    base_phase = cur[0]

    wseq = []
    wstate = {"i": 0, "issued": 0, "rec": True}
    PREF = 2

    def wget(parts, keep=False):
        i = wstate["i"]; wstate["i"] += 1
        if wstate["rec"]:
            wseq.append(parts)
            return wb[i % NWB]
        live = wstate.setdefault("live", set())
        if not keep:
            live.clear()
        live.add(i)
        while wstate["issued"] < min(len(wseq), i + 1 + PREF):
            j = wstate["issued"]
            if any((x % NWB) == (j % NWB) and x != j for x in live):
                break
            wstate["issued"] += 1
            for (dst_fn, src, rkeys) in wseq[j]:
                DMA(dst_fn(wb[j % NWB]), src, r=rkeys, q="pool")
        assert wstate["issued"] > i, "weight slot conflict: too many live items"
        return wb[i % NWB]

    def wpart(mat, r0, nk, c0, ncol, k0=0, cc0=0):
        src = mat[r0:r0 + nk * 128, c0:c0 + ncol].rearrange("(k p) n -> p k n", p=128)
        return (lambda s: s.rearrange("p (k n) -> p k n", k=KQ)[:, k0:k0 + nk, cc0:cc0 + ncol], src, ())

    def body():
        cur[0] = base_phase
        psn[0] = 0
        DMA(ident, ident_d[:, :])
        DMA(shm, shm_d.rearrange("i p m -> p i m"))
        CP(identb, ident)
        MS(onesb, 1.0)
        o_c = alloc(D); ctile = f32v(o_c, D)
        DMA(ctile[0:17, :], cvec[:, :])
        ACT(ctile[0:17, :], ctile[0:17, :], AF.Silu)
        for g4 in range(4):
            pb = nb()
            for q in range(4):
                kq = g4 * 4 + q
                TR(pb[:, q * 17:(q + 1) * 17], ctile[0:17, kq * 128:(kq + 1) * 128], ident[0:17, 0:17])
            CP(scT[:, g4 * 4:(g4 + 1) * 4, :], pb[:, 0:68].rearrange("p (a b) -> p a b", a=4))
        cur[0] = o_c

        for pas in passes:
            run_pass(pas)

    def run_pass(pas):
        isA = (pas == "A")
        NTc = NT if isA else NP
        CT = [(0, 512), (512, 512)] + ([(1024, 128)] if isA else [])
        seg = 0 if isA else 1
        p0 = cur[0]

        def dbg(name, ap_sb):
            if debug and (pas + "_" + name) in dbg_o:
                DMA(dbg_o[pas + "_" + name], ap_sb)

        o_r = alloc(KQ * NT); rbuf = f32v(o_r, KQ * NT).rearrange("p (k n) -> p k n", k=KQ)
        o_xt = alloc(2 * D); xt = [f32v(o_xt, D), f32v(o_xt + D, D)]
        xseg = xp[seg * NP:(seg + 1) * NP, :].rearrange("(c j) d -> j c d", j=8)
        for j in range(9 if isA else 8):
            t = xt[j % 2]
            if j < 8:
                DMA(t, xseg[j])
            else:
                for jj in range(8):
                    DMA(t[jj * 16:(jj + 1) * 16, :], xs[:, jj, :])
            for g4 in range(4):
                pb = nb()
                for q in range(4):
                    kq = g4 * 4 + q
                    TR(pb[:, q * 128:(q + 1) * 128], t[:, kq * 128:(kq + 1) * 128], ident)
                ACT(rbuf[:, g4 * 4:(g4 + 1) * 4, j * 128:(j + 1) * 128], pb.rearrange("p (a b) -> p a b", a=4), AF.Copy, scale=ALPHA)
        cur[0] = o_xt

        def ln_alloc():
            d = {}
            d["mean"] = f32v(alloc(NT), NT); d["rstd"] = f32v(alloc(NT), NT)
            d["tmpA"] = f32v(alloc(NT), NT); d["tmpB"] = f32v(alloc(128), 128)
            d["tmpA2"] = [d["tmpA"], f32v(alloc(NT), NT)]
            d["tb"] = []
            for _ in range(2):
                o = alloc(NT); d["tb"].append((bf16v(o, NT), bf16v(o + NT // 2, NT)))
            return d

        def ln_stats(L, src_fn, nch, eps):
            banks = [nb() for _ in range(2 * len(CT))]
            for k in range(nch):
                tb1, tb2 = L["tb"][k % 2]
                CP(tb1[:, 0:NTc], src_fn(k))
                ACT(tb2[:, 0:NTc], src_fn(k), AF.Square)
                for ci, (c0, cn) in enumerate(CT):
                    MM(banks[ci][:, 0:cn], onesb, tb1[:, c0:c0 + cn], k == 0, k == nch - 1)
                    MM(banks[len(CT) + ci][:, 0:cn], onesb, tb2[:, c0:c0 + cn], k == 0, k == nch - 1)
            nf = float(nch * 128)
            for ci, (c0, cn) in enumerate(CT):
                TS(L["mean"][:, c0:c0 + cn], banks[ci][:, 0:cn], 1.0 / nf, None, ALU.mult)
                TS(L["rstd"][:, c0:c0 + cn], banks[len(CT) + ci][:, 0:cn], 1.0 / nf, None, ALU.mult)
            m = L["mean"][:, 0:NTc]; r_ = L["rstd"][:, 0:NTc]; ta = L["tmpA"][:, 0:NTc]
            TT(ta, m, m, ALU.mult)
            TT(r_, r_, ta, ALU.subtract)
            TS(r_, r_, eps, None, ALU.add)
            ACT(r_, r_, AF.Sqrt)
            RCP(r_, r_)

        def modulate_to_h(L, part_sc, part_sh):
            m = L["mean"][:, 0:NTc]; r_ = L["rstd"][:, 0:NTc]
            for k in range(KQ):
                ta = L["tmpA2"][k % 2]
                (TTP if k % 2 == 0 else TT)(ta[:, 0:NTc], rbuf[:, k, 0:NTc], m, ALU.subtract)
                TT(ta[:, 0:NTc], ta[:, 0:NTc], r_, ALU.mult)
                sc = adaT[:, part_sc * 16 + k, :]; sh = adaT[:, part_sh * 16 + k, :]
                ACT(hT[:, k, 0:NP], ta[:, 0:NP], AF.Identity, scale=sc[:, 0:1], bias=sh[:, 0:1])
                if isA:
                    t3 = ta[:, NP:NT].rearrange("p (j s) -> p j s", j=8)
                    TT(t3, t3, sc[:, 1:17].unsqueeze(1).to_broadcast([128, 8, 16]), ALU.mult)
                    TT(hT[:, k, NP:NT].rearrange("p (j s) -> p j s", j=8), t3, sh[:, 1:17].unsqueeze(1).to_broadcast([128, 8, 16]), ALU.add)

        def resid_add(k, ci, pb, gpart, tmpB):
            c0, cn = CT[ci]
            g = adaT[:, gpart * 16 + k, :]
            if ci < 2:
                STT(rbuf[:, k, c0:c0 + cn], pb[:, 0:cn], g[:, 0:1], rbuf[:, k, c0:c0 + cn], ALU.mult, ALU.add)
            else:
                t3 = tmpB.rearrange("p (j s) -> p j s", j=8)
                TT(t3, pb[:, 0:128].rearrange("p (j s) -> p j s", j=8), g[:, 1:17].unsqueeze(1).to_broadcast([128, 8, 16]), ALU.mult)
                TT(rbuf[:, k, c0:c0 + cn], rbuf[:, k, c0:c0 + cn], tmpB, ALU.add)

        def ln_affine_inplace(L, goff):
            m = L["mean"][:, 0:NTc]; r_ = L["rstd"][:, 0:NTc]
            for k in range(KQ):
                x_ = rbuf[:, k, 0:NTc]
                (TTP if k % 2 == 0 else TT)(x_, x_, m, ALU.subtract)
                TT(x_, x_, r_, ALU.mult)
                ACT(x_, x_, AF.Identity, scale=gA[:, goff + k:goff + k + 1], bias=gA[:, goff + 16 + k:goff + 17 + k])

        def ada_bias_load(la):
            stb = f32v(alloc(128), 128)
            DMA(stb[0:96, :], b_ada[la].rearrange("(c p) -> c p", p=128))
            pb_ = nb(); TR(pb_[:, 0:96], stb[0:96, :], ident[0:96, 0:96]); CP(badaN, pb_[:, 0:96])
            cur[0] -= 128

        def ada_block(la, blk):
            wv_ = wget([wpart(w_ada[la], 0, KQ, blk * 512, 512)]).rearrange("p (k n) -> p k n", k=KQ)
            stg_ = astage[blk % 2].rearrange("p (c b) -> p c b", c=4)
            for m in range(4):
                ch = blk * 4 + m
                pb_ = nb()
                for k in range(KQ):
                    MM(pb_[:, 0:17], wv_[:, k, m * 128:(m + 1) * 128], scT[:, k, :], k == 0, k == KQ - 1)
                if ch // 16 in (1, 4):
                    TS(stg_[:, m, :], pb_[:, 0:17], badaN[:, ch:ch + 1], 1.0, ALU.add, ALU.add)
                else:
                    TS(stg_[:, m, :], pb_[:, 0:17], badaN[:, ch:ch + 1], None, ALU.add)
            DMA(ada_scr[la][:, blk * 68:(blk + 1) * 68], astage[blk % 2], w=[("ada_scr", la)])

        phase0 = cur[0]
        if isA:
            ada_bias_load(0)
            for blk in range(24):
                ada_block(0, blk)
        for l in range(depth):
            cur[0] = phase0
            last = (l == DEPTH - 1)
            o_st = alloc(128); st = f32v(o_st, 128)
            pvw = lambda v: v.rearrange("(c p) -> c p", p=128)
            DMA(st[0:96, :], pvw(b_ada[l])); DMA(st[96:112, :], pvw(ln1_g_d[l])); DMA(st[112:128, :], pvw(ln1_b_d[l]))
            pb = nb(); TR(pb[:, 0:128], st, ident); CP(pv1, pb[:, 0:128])
            for i_, src in enumerate([ln2_g_d, ln2_b_d]):
                DMA(st[i_ * 16:(i_ + 1) * 16, :], pvw(src[l]))
            for i_, src in enumerate([conv_b_d, cln_g_d, cln_b_d, ssm_d_d]):
                DMA(st[32 + i_ * 8:32 + (i_ + 1) * 8, :], pvw(src[l]))
            pb = nb(); TR(pb[:, 0:64], st[0:64, :], ident[0:64, 0:64]); CP(pv2, pb[:, 0:64])
            cwv = conv_w_d[l].rearrange("w (q p) -> (w q) p", p=128)
            DMA(st[0:128, :], cwv[0:128, :])
            pb = nb(); TR(pb[:, 0:128], st, ident); CP(cw[:, 0:128], pb[:, 0:128])
            DMA(st[0:120, :], cwv[128:248, :])
            pb = nb(); TR(pb[:, 0:120], st[0:120, :], ident[0:120, 0:120]); CP(cw[:, 128:248], pb[:, 0:120])
            TS(gA[:, 0:32], pv1[:, 96:128], ALPHA, None, ALU.mult)
            TS(gA[:, 32:64], pv2[:, 0:32], (1.0 if last else ALPHA), None, ALU.mult)
            DMA(rbias[:, 0:4], mbg_d[l].partition_broadcast(128))
            DMA(rbias[:, 4:36], mbr_d[l].partition_broadcast(128))
            cur[0] = o_st

            adaflat = adaT.rearrange("p c b -> p (c b)")
            DMA(adaflat, ada_scr[l], r=[("ada_scr", l)])

            L = ln_alloc()
            ln_stats(L, lambda k: rbuf[:, k, 0:NTc], KQ, ALPHA * ALPHA * LN_EPS)
            modulate_to_h(L, 1, 0)
            for k in range(KQ):
                DMA(xscr[k][:, 0:NTc], rbuf[:, k, 0:NTc], w=[("xscr", k)])
            if l == 0: dbg("h1", hT[:, :, 0:NTc])
            cur[0] = o_r

            if isA:
                ssm_prep(l)
            DMA(A8, A8_scr[l], r=[("A8_scr", l)])
            A8r = A8[:, 0:32]; A8i = A8[:, 32:64]
            cur[0] = o_r

            ysT = bf16v(alloc(8 * NT // 2), 8 * NT).rearrange("p (q n) -> p q n", q=8)
            ph1 = cur[0]
            Ucm = bf16v(alloc(4096), 8192).rearrange("p (g j k) -> p g j k", g=64, j=8)
            Um = bf16v(alloc(64 * 144 // 2), 64 * 144).rearrange("p (g c) -> p g c", g=64)
            Vm = f32v(alloc(2 * 32 * 145), 2 * 32 * 145).rearrange("p (r g c) -> p r g c", r=2, g=32)
            NCH = 144 if isA else 128
            wslots = []
            for half in range(2):
                wv = wget([wpart(w_in[l], 0, KQ, half * 512, 512)]).rearrange("p (k n) -> p k n", k=KQ)
                wslots.append(wv)
                for j in range(8):
                    pb = nb()
                    for k in range(KQ):
                        MM(pb[:, 0:512], hT[:, k, j * 128:(j + 1) * 128], wv[:, k, :], k == 0, k == KQ - 1)
                    ACP(Ucm[:, half * 32:(half + 1) * 32, j, :], pb[:, 0:512].rearrange("p (g k) -> p g k", g=32))
                if isA:
                    pass
            for g4 in range(16):
                pb = nb()
                pbb = pb[:, 0:256].bitcast(BF16).rearrange("p (g c) -> p g c", g=4)
                for q in range(4):
                    TR(pbb[:, q, :], Ucm[:, g4 * 4 + q].rearrange("p j k -> p (j k)"), identb)
                ACP(Um[:, g4 * 4:(g4 + 1) * 4, 0:128], pbb)
            if isA:
                for half in range(2):
                    wv = wget([wpart(w_in[l], 0, KQ, half * 512, 512)]).rearrange("p (k n) -> p k n", k=KQ)
                    for j in range(8):
                        pb = nb()
                        for k in range(KQ):
                            MM(pb[0:16, 0:512], hT[:, k, NP + j * 16:NP + (j + 1) * 16], wv[:, k, :], k == 0, k == KQ - 1)
                        ACT(Ucm[0:16, half * 32:(half + 1) * 32, j, :], pb[0:16, 0:512].rearrange("p (g k) -> p g k", g=32), AF.Copy)
                for g4 in range(16):
                    pb = nb()
                    pbb = pb[:, 0:32].bitcast(BF16).rearrange("p (g c) -> p g c", g=4)
                    for q in range(4):
                        TR(pbb[:, q, :], Ucm[0:16, g4 * 4 + q].rearrange("p j k -> p (j k)"), identb[0:16, 0:16])
                    ACP(Um[:, g4 * 4:(g4 + 1) * 4, 128:144], pbb)
            Rw = wget([(lambda s: s, R_scr[l], [("R_scr", l)])]).rearrange("p (g r q) -> p g r q", g=64, r=2)
            for gg in range(32):
                pb = nb()
                for ri_ in range(2):
                    for gh in range(2):
                        g = gh * 32 + gg
                        MM(pb[gh * 64:(gh + 1) * 64, ri_ * NCH:(ri_ + 1) * NCH], Rw[:, g, ri_, :], Um[:, g, 0:NCH], True, True)
                ACP(Vm[:, :, gg, 1:1 + NCH], pb[:, 0:2 * NCH].rearrange("p (r c) -> p r c", r=2))
            if isA:
                MS(Vm[:, :, :, 0], 0.0)
            else:
                CP(Vm[:, :, :, 0], stsave[:, l, :, :])
            stgA = f32v(alloc(128), 128); stgB = f32v(alloc(128), 128); ytmp = f32v(alloc(512), 512)
            s_t1 = f32v(alloc(64), 64).rearrange("p (r g) -> p r g", r=2)
            s_t2 = f32v(alloc(64), 64).rearrange("p (r g) -> p r g", r=2)
            A8rb = A8r.unsqueeze(1).to_broadcast([128, 2, 32])

            def cstep(prev, curc):
                TT(s_t1, prev, A8rb, ALU.mult)
                TT(s_t2[:, 0, :], prev[:, 1, :], A8i, ALU.mult)
                TT(s_t2[:, 1, :], prev[:, 0, :], A8i, ALU.mult)
                TT(s_t1, s_t1, curc, ALU.add)
                TT(curc[:, 0, :], s_t1[:, 0, :], s_t2[:, 0, :], ALU.subtract)
                TT(curc[:, 1, :], s_t1[:, 1, :], s_t2[:, 1, :], ALU.add)
            for c in range(128):
                cstep(Vm[:, :, :, c], Vm[:, :, :, c + 1])
            if isA:
                CP(stsave[:, l, :, :], Vm[:, :, :, 128])
            if (not isA) or ("B" not in passes):
                for ri_, od in enumerate([pre_o, pim_o]):
                    pb = nb()
                    TR(pb[0:32, 0:128], Vm[:, ri_, :, 128], ident)
                    CP(stgA[0:32, :], pb[0:32, 0:128])
                    DMA(od[l].rearrange("(h g) p -> g h p", h=2), stgA[0:32, :].rearrange("g (h p) -> g h p", h=2))
            if isA:
                Sis = f32v(alloc(1024), 1024).rearrange("p (r g s) -> p r g s", r=2, g=32)
                for ri_, sd in enumerate([sre_i, sim_i]):
                    for s4 in range(4):
                        sst = stgA
                        for gh in range(2):
                            for s1 in range(4):
                                DMA(sst[s1 * 32:(s1 + 1) * 32, gh * 64:(gh + 1) * 64], sd[l][s4 * 4 + s1, gh * 32:(gh + 1) * 32, :])
                        pb = nb()
                        TR(pb[:, 0:128], sst, ident)
                        CP(Sis[:, ri_, :, s4 * 4:(s4 + 1) * 4], pb[:, 0:128].rearrange("p (s g) -> p g s", s=4))
                s16a = f32v(alloc(1024), 1024).rearrange("p (r g s) -> p r g s", r=2, g=32)
                s16t = ytmp.rearrange("p (g s) -> p g s", g=32)
                b16 = lambda a: a.unsqueeze(2).to_broadcast([128, 32, 16])
                vs = Vm[:, :, :, 129:145]
                TT(s16a[:, 0], Sis[:, 0], b16(A8r), ALU.mult)
                TT(s16t, Sis[:, 1], b16(A8i), ALU.mult)
                TT(s16a[:, 0], s16a[:, 0], s16t, ALU.subtract)
                TT(s16a[:, 1], Sis[:, 1], b16(A8r), ALU.mult)
                TT(s16t, Sis[:, 0], b16(A8i), ALU.mult)
                TT(s16a[:, 1], s16a[:, 1], s16t, ALU.add)
                TT(vs, vs, s16a, ALU.add)
                for ri_, od in enumerate([sre_o, sim_o]):
                    for s4 in range(4):
                        pb = nb()
                        stg = stgA; stg2 = stgB
                        CP(stg.rearrange("p (s g) -> p s g", s=4), Vm[:, ri_, :, 129 + s4 * 4:133 + s4 * 4].rearrange("p g s -> p s g"))
                        TR(pb[:, 0:128], stg, ident)
                        CP(stg2, pb[:, 0:128])
                        for gh in range(2):
                            for s1 in range(4):
                                DMA(od[l][s4 * 4 + s1, gh * 32:(gh + 1) * 32, :], stg2[s1 * 32:(s1 + 1) * 32, gh * 64:(gh + 1) * 64])
            Tw = wget([(lambda s: s, T_scr[l], [("T_scr", l)])]).rearrange("p (g n) -> p g n", g=64)
            Ow = [wget([(lambda s: s.bitcast(F32), O_scr[l][ri_], [("O_scr", l)])], keep=True).bitcast(F32).rearrange("p (g n) -> p g n", g=32) for ri_ in range(2)]
            Vf = Vm
            Ycm = Ucm.rearrange("p g j k -> p (g j k)").rearrange("p (i n) -> p i n", i=8)
            for g4 in range(16):
                pbT = nb(); pbO = nb()
                for q in range(4):
                    g = g4 * 4 + q; gh = g // 32; gg = g % 32
                    sl = slice(gh * 64, (gh + 1) * 64)
                    MM(pbT[:, q * 128:(q + 1) * 128], Um[:, g, 0:128], Tw[:, g, :], True, True)
                    MM(pbO[:, q * 128:(q + 1) * 128], Vf[sl, 0, gg, 0:128], Ow[0][sl, gg, :], True, False)
                    MM(pbO[:, q * 128:(q + 1) * 128], Vf[sl, 1, gg, 0:128], Ow[1][sl, gg, :], False, True)
                ACP(ytmp, pbT[:, 0:512])
                TT(Ycm[:, :, g4 * 64:(g4 + 1) * 64].rearrange("p i (g k) -> p g i k", g=4), ytmp.rearrange("p (g i k) -> p g i k", g=4, i=8),
                   pbO[:, 0:512].rearrange("p (g i k) -> p g i k", g=4, i=8), ALU.add)
            for q in range(8):
                pb = nb()
                pbb = pb[:, 0:512].bitcast(BF16).rearrange("p (i c) -> p i c", i=8)
                for i in range(8):
                    TR(pbb[:, i, :], Ycm[:, i, q * 128:(q + 1) * 128], identb)
                ACT(ysT[:, q, 0:NP], pb[:, 0:512].bitcast(BF16), AF.Gelu_apprx_tanh)
            if isA:
                for g4 in range(16):
                    pbT = nb(); pbO = nb()
                    for q in range(4):
                        g = g4 * 4 + q; gh = g // 32; gg = g % 32
                        sl = slice(gh * 64, (gh + 1) * 64)
                        MM(pbT[0:16, q * 128:(q + 1) * 128], Um[:, g, 128:144], Tw[:, g, :], True, True)
                        MM(pbO[0:16, q * 128:(q + 1) * 128], Sis[sl, 0, gg, :], Ow[0][sl, gg, :], True, False)
                        MM(pbO[0:16, q * 128:(q + 1) * 128], Sis[sl, 1, gg, :], Ow[1][sl, gg, :], False, True)
                    CP(ytmp[0:16, :], pbT[0:16, 0:512])
                    TT(Ycm[0:16, :, g4 * 64:(g4 + 1) * 64].rearrange("p i (g k) -> p g i k", g=4), ytmp[0:16, :].rearrange("p (g i k) -> p g i k", g=4, i=8),
                       pbO[0:16, 0:512].rearrange("p (g i k) -> p g i k", g=4, i=8), ALU.add)
                for q in range(8):
                    pb = nb()
                    pbb = pb[:, 0:64].bitcast(BF16).rearrange("p (i c) -> p i c", i=8)
                    for i in range(8):
                        TR(pbb[:, i, :], Ycm[0:16, i, q * 128:(q + 1) * 128], identb[0:16, 0:16])
                    ACT(ysT[:, q, NP:NT], pb[:, 0:64].bitcast(BF16), AF.Gelu_apprx_tanh)
            if l == 0: dbg("ysT", ysT[:, :, 0:NTc])
            if l == 0 and debug:
                dbg("Ycm", Ycm); dbg("Um", Um); dbg("Vm", Vm)
                for nm, scr in (("Tscr", T_scr[l]), ("Rscr", R_scr[l]), ("O0", O_scr[l][0]), ("O1", O_scr[l][1])):
                    if (pas + "_" + nm) in dbg_o:
                        DMA(dbg_o[pas + "_" + nm], scr, r=[("T_scr", l), ("O_scr", l), ("R_scr", l)])
            if stop_after == "ssm":
                return
            cur[0] = ph1

            vT = bf16v(alloc(8 * NT // 2), 8 * NT).rearrange("p (q n) -> p q n", q=8)
            ph2 = cur[0]
            vbuf = f32v(alloc(8 * NT), 8 * NT).rearrange("p (q n) -> p q n", q=8)
            ph3 = cur[0]
            hist = f32v(alloc(240), 240).rearrange("p (q t) -> p q t", q=8)
            ubpb_l = [bf16v(alloc(528), 1056)[:, 0:1054] for _ in range(2)]
            ubsf_l = [f32v(alloc(608), 608).rearrange("p (s t) -> p s t", s=16) for _ in range(2)]
            ubsb_l = [bf16v(alloc(304), 608).rearrange("p (s t) -> p s t", s=16) for _ in range(2)]
            tailf_l = [f32v(alloc(32), 32) for _ in range(2)]
            dg = [bf16v(alloc(64), 128) for _ in range(31)]
            sg_t = f32v(alloc(512), 512)
            if isA:
                MS(hist, 0.0)
            else:
                CP(hist, cvsave[:, l, :, :])
            for q in range(8):
                ubpb = ubpb_l[q % 2]; ubsf = ubsf_l[q % 2]; ubsb = ubsb_l[q % 2]; tailf = tailf_l[q % 2]
                wv = wget([wpart(w_in[l], 0, KQ, 1024 + q * 128, 128, 0, 0), wpart(w_in[l], 0, KQ, 2048 + q * 128, 128, 0, 128)]).rearrange("p (k n) -> p k n", k=KQ)
                if isA:
                    for s4 in range(4):
                        sst = f32v(alloc(128), 128)
                        DMA(sst[0:120, :], scv_i[l][s4 * 4:(s4 + 1) * 4, :, q * 128:(q + 1) * 128].rearrange("s t c -> (s t) c"))
                        pb = nb()
                        TR(pb[:, 0:120], sst[0:120, :], ident[0:120, 0:120])
                        CP(ubsf[:, s4 * 4:(s4 + 1) * 4, 0:30], pb[:, 0:120].rearrange("p (s t) -> p s t", s=4))
                        cur[0] -= 128
                CP(ubpb[:, 0:30], hist[:, q, :])
                for w_ in range(31):
                    TSP(dg[w_], identb, cw[:, w_ * 8 + q:w_ * 8 + q + 1])
                for ci, (c0, cn) in enumerate(CT):
                    pa = nb(); pbb_ = nb()
                    for k in range(KQ):
                        MM(pa[:, 0:cn], wv[:, k, 0:128], hT[:, k, c0:c0 + cn], k == 0, k == KQ - 1)
                    for k in range(KQ):
                        MM(pbb_[:, 0:cn], wv[:, k, 128:256], hT[:, k, c0:c0 + cn], k == 0, k == KQ - 1)
                    ACT(sg_t[:, 0:cn], pbb_[:, 0:cn], AF.Sigmoid)
                    if ci < 2:
                        pav = pa[:, 0:512].rearrange("p (j c) -> p j c", j=4); sgv = sg_t.rearrange("p (j c) -> p j c", j=4)
                        dst = ubpb[:, 30:1054].rearrange("p (c j) -> p j c", j=8)[:, ci * 4:(ci + 1) * 4, :]
                        TT(dst, pav, sgv, ALU.mult)
                        TT(tailf.rearrange("p (c j) -> p j c", j=8)[:, ci * 4:(ci + 1) * 4, :], pav[:, :, 124:128], sgv[:, :, 124:128], ALU.mult)
                    else:
                        dst = ubsf[:, :, 30:38].rearrange("p s j -> p j s")
                        TT(dst, pa[:, 0:128].rearrange("p (j s) -> p j s", j=8), sg_t[:, 0:128].rearrange("p (j s) -> p j s", j=8), ALU.mult)
                        CP(ubsb, ubsf)
                for tt in range(2):
                    pv = nb()
                    for w_ in range(31):
                        MM(pv[:, 0:512], dg[w_], ubpb[:, w_ + tt * 512:w_ + tt * 512 + 512], w_ == 0, w_ == 30)
                    ACT(vbuf[:, q, tt * 512:(tt + 1) * 512], pv[:, 0:512], AF.Identity, bias=pv2[:, 32 + q:33 + q])
                if isA:
                    pv = nb()
                    for w_ in range(31):
                        MM(pv[:, 0:128].rearrange("p (s j) -> p s j", s=16), dg[w_], ubsb[:, :, w_:w_ + 8], w_ == 0, w_ == 30)
                    ACT(vbuf[:, q, NP:NT], pv[:, 0:128], AF.Identity, bias=pv2[:, 32 + q:33 + q])
                    CP(cvsave[:, l, q, :], tailf[:, 2:32])
                    for s4 in range(4):
                        stg = f32v(alloc(128), 128)
                        CP(stg[:, 0:120].rearrange("p (s t) -> p s t", s=4), ubsf[:, s4 * 4:(s4 + 1) * 4, 8:38])
                        pb = nb()
                        TR(pb[0:120, 0:128], stg[:, 0:120], ident)
                        stg2 = f32v(alloc(128), 128)
                        CP(stg2[0:120, :], pb[0:120, 0:128])
                        DMA(scv_o[l][s4 * 4:(s4 + 1) * 4, :, q * 128:(q + 1) * 128].rearrange("s t c -> (s t) c"), stg2[0:120, :])
                        cur[0] -= 256
                if (not isA) or ("B" not in passes):
                    stg = f32v(alloc(128), 128)
                    pb = nb()
                    TR(pb[0:30, 0:128], tailf[:, 2:32], ident)
                    CP(stg[0:30, :], pb[0:30, 0:128])
                    DMA(pcv_o[l][:, q * 128:(q + 1) * 128], stg[0:30, :])
                    cur[0] -= 128
            cur[0] = ph3
            L = ln_alloc()
            ln_stats(L, lambda k: vbuf[:, k, 0:NTc], 8, LN_EPS)
            m = L["mean"][:, 0:NTc]; r_ = L["rstd"][:, 0:NTc]
            for q in range(8):
                x_ = vbuf[:, q, 0:NTc]
                TT(x_, x_, m, ALU.subtract)
                TT(x_, x_, r_, ALU.mult)
                ACT(x_, x_, AF.Identity, scale=pv2[:, 40 + q:41 + q], bias=pv2[:, 48 + q:49 + q])
                ACT(vT[:, q, 0:NP].rearrange("p (j c) -> p c j", j=8), vbuf[:, q, 0:NP].rearrange("p (c j) -> p c j", j=8), AF.Silu)
                if isA:
                    ACT(vT[:, q, NP:NT].rearrange("p (j s) -> p s j", j=8), vbuf[:, q, NP:NT].rearrange("p (s j) -> p s j", s=16), AF.Silu)
            if l == 0: dbg("vT", vT[:, :, 0:NTc])

            cur[0] = ph2
            mix = bf16v(alloc(KQ * NT // 2), KQ * NT).rearrange("p (k n) -> p k n", k=KQ)
            tAB = [(f32v(alloc(512), 512), f32v(alloc(512), 512)) for _ in range(2)]
            tcnt = [0]
            for mb in range(8):
                wv = wget([wpart(w_in[l], 0, KQ, 3072 + mb * 256, 256, 0, 0), wpart(w_val[l], 0, 8, mb * 256, 256, 0, 256),
                           wpart(w_gate[l], 0, 8, mb * 256, 256, 8, 256)]).rearrange("p (k n) -> p k n", k=KQ)
                for m_ in range(2):
                    mch = mb * 2 + m_
                    for ci, (c0, cn) in enumerate(CT):
                        p1 = nb(); p2 = nb(); p3 = nb()
                        tA, tB = tAB[tcnt[0] % 2]; tcnt[0] += 1
                        for k in range(8):
                            MM(p1[:, 0:cn], wv[:, k, 256 + m_ * 128:256 + (m_ + 1) * 128], ysT[:, k, c0:c0 + cn], k == 0, k == 7)
                        for k in range(8):
                            MM(p2[:, 0:cn], wv[:, 8 + k, 256 + m_ * 128:256 + (m_ + 1) * 128], ysT[:, k, c0:c0 + cn], k == 0, k == 7)
                        for k in range(KQ):
                            MM(p3[:, 0:cn], wv[:, k, m_ * 128:(m_ + 1) * 128], hT[:, k, c0:c0 + cn], k == 0, k == KQ - 1)
                        ACT(tA[:, 0:cn], p2[:, 0:cn], AF.Sigmoid)
                        ACT(tB[:, 0:cn], p3[:, 0:cn], AF.Sigmoid)
                        TT(tA[:, 0:cn], tA[:, 0:cn], p1[:, 0:cn], ALU.mult)
                        TT(mix[:, mch, c0:c0 + cn], tA[:, 0:cn], tB[:, 0:cn], ALU.mult)
            for mb in range(8):
                wv = wget([wpart(w_in[l], 0, KQ, 5120 + mb * 256, 256, 0, 0), wpart(w_pw[l], 0, 8, mb * 256, 256, 0, 256)]).rearrange("p (k n) -> p k n", k=KQ)
                for m_ in range(2):
                    mch = mb * 2 + m_
                    for ci, (c0, cn) in enumerate(CT):
                        p1 = nb(); p3 = nb()
                        tA, tB = tAB[tcnt[0] % 2]; tcnt[0] += 1
                        for k in range(8):
                            MM(p1[:, 0:cn], wv[:, k, 256 + m_ * 128:256 + (m_ + 1) * 128], vT[:, k, c0:c0 + cn], k == 0, k == 7)
                        for k in range(KQ):
                            MM(p3[:, 0:cn], wv[:, k, m_ * 128:(m_ + 1) * 128], hT[:, k, c0:c0 + cn], k == 0, k == KQ - 1)
                        ACT(tB[:, 0:cn], p3[:, 0:cn], AF.Sigmoid)
                        TT(tB[:, 0:cn], tB[:, 0:cn], p1[:, 0:cn], ALU.mult)
                        TT(mix[:, mch, c0:c0 + cn], mix[:, mch, c0:c0 + cn], tB[:, 0:cn], ALU.add)
            if l == 0: dbg("mix", mix[:, :, 0:NTc])
            for k in range(KQ):
                CP(hT[:, k, 0:NTc], mix[:, k, 0:NTc])
            mix = hT
            cur[0] = o_r + KQ * NT
            tmpB = f32v(alloc(128), 128)
            for k in range(KQ):
                DMA(rbuf[:, k, 0:NTc], xscr[k][:, 0:NTc], r=[("xscr", k)])
            for mb in range(4):
                wv = wget([wpart(w_out[l], 0, KQ, mb * 512, 512)]).rearrange("p (k n) -> p k n", k=KQ)
                for m_ in range(4):
                    mch = mb * 4 + m_
                    for ci, (c0, cn) in enumerate(CT):
                        pb = nb()
                        for k in range(KQ):
                            MM(pb[:, 0:cn], wv[:, k, m_ * 128:(m_ + 1) * 128], mix[:, k, c0:c0 + cn], k == 0, k == KQ - 1)
                        resid_add(mch, ci, pb, 2, tmpB)
            cur[0] = o_r + KQ * NT
            L = ln_alloc()
            ln_stats(L, lambda k: rbuf[:, k, 0:NTc], KQ, LN_EPS)
            ln_affine_inplace(L, 0)
            if l == 0: dbg("x1", rbuf[:, :, 0:NTc])
            ln_stats(L, lambda k: rbuf[:, k, 0:NTc], KQ, ALPHA * ALPHA * LN_EPS)
            modulate_to_h(L, 4, 3)
            cur[0] = o_r + KQ * NT

            NTILE = NTc // 128
            comb = f32v(alloc(9 * 32), 288).rearrange("p (t e) -> p t e", t=9)
            combT = f32v(alloc(NT), NT)
            eselc = f32v(alloc(128), 128)
            lg = f32v(alloc(36), 36); m8 = f32v(alloc(8), 8); sm = f32v(alloc(16), 16)
            em = f32v(alloc(32), 32); msk = f32v(alloc(32), 32); gmk = f32v(alloc(4), 4); ge = f32v(alloc(4), 4)
            wr = wget([(lambda s: s.rearrange("p (k n) -> p k n", k=KQ)[:, :, 0:4], mwg_d[l].rearrange("(k p) n -> p k n", p=128), ()),
                       (lambda s: s.rearrange("p (k n) -> p k n", k=KQ)[:, :, 4:36], mwr_d[l].rearrange("(k p) n -> p k n", p=128), ())]).rearrange("p (k n) -> p k n", k=KQ)
            for t in range(NTILE):
                pb = nb()
                for k in range(KQ):
                    MM(pb[:, 0:36], hT[:, k, t * 128:(t + 1) * 128], wr[:, k, 0:36], k == 0, k == KQ - 1)
                TT(lg, pb[:, 0:36], rbias, ALU.add)
                glog = lg[:, 0:4]; elog = lg[:, 4:36]
                P.op("dve", (lambda o_=sm[:, 0:1], i_=glog: nc.vector.reduce_max(out=o_, in_=i_, axis=mybir.AxisListType.X)), ins=[glog], outs=[sm[:, 0:1]])
                TS(gmk, glog, sm[:, 0:1], None, ALU.is_equal)
                TS(sm[:, 1:2], sm[:, 0:1], -1.0, None, ALU.mult)
                ACT(ge, glog, AF.Exp, bias=sm[:, 1:2])
                P.op("dve", (lambda o_=sm[:, 2:3], i_=ge: nc.vector.reduce_sum(out=o_, in_=i_, axis=mybir.AxisListType.X)), ins=[ge], outs=[sm[:, 2:3]])
                RCP(sm[:, 3:4], sm[:, 2:3])
                TS(gmk, gmk, 1e30, -1e30, ALU.mult, ALU.add)
                TT(em.rearrange("p (g e) -> p g e", g=4), elog.rearrange("p (g e) -> p g e", g=4), gmk.unsqueeze(2).to_broadcast([128, 4, 8]), ALU.add)
                P.op("dve", (lambda o_=m8, i_=em: nc.vector.max(out=o_, in_=i_)), ins=[em], outs=[m8])
                TT(sm[:, 4:5], m8[:, 1:2], m8[:, 0:1], ALU.subtract)
                ACT(sm[:, 5:6], sm[:, 4:5], AF.Exp)
                TS(sm[:, 6:7], sm[:, 5:6], 1.0, None, ALU.add)
                RCP(sm[:, 7:8], sm[:, 6:7])
                TT(sm[:, 8:9], sm[:, 7:8], sm[:, 5:6], ALU.mult)
                TT(sm[:, 7:8], sm[:, 7:8], sm[:, 3:4], ALU.mult)
                TT(sm[:, 8:9], sm[:, 8:9], sm[:, 3:4], ALU.mult)
                TS(msk, em, m8[:, 0:1], sm[:, 7:8], ALU.is_equal, ALU.mult)
                TS(comb[:, t, :], em, m8[:, 1:2], sm[:, 8:9], ALU.is_equal, ALU.mult)
                TT(comb[:, t, :], comb[:, t, :], msk, ALU.add)
                pb = nb()
                TR(pb[0:32, 0:128], comb[:, t, :], ident)
                CP(combT[0:32, t * 128:(t + 1) * 128], pb[0:32, 0:128])
            if l == 0: dbg("combT", combT[0:32, 0:NTc])
            EP = 4
            act = bf16v(alloc(EP * 2 * NT // 2), EP * 2 * NT).rearrange("p (a n) -> p a n", a=EP * 2)
            tA = f32v(alloc(512), 512); tB = f32v(alloc(512), 512); tmpB = f32v(alloc(128), 128)
            do_next_ada = isA and (l + 1 < depth)
            if do_next_ada:
                ada_bias_load(l + 1)
            for ep in range(32 // EP):
                if do_next_ada:
                    for blk in range(ep * 3, ep * 3 + 3):
                        ada_block(l + 1, blk)
                for el in range(EP):
                    e = ep * EP + el
                    wv = wget([(lambda s: s.rearrange("p (k n) -> p k n", k=KQ)[:, :, 0:256], w_up[l, e].rearrange("(k p) n -> p k n", p=128), ()),
                               (lambda s: s.rearrange("p (k n) -> p k n", k=KQ)[:, :, 256:512], w_gt[l, e].rearrange("(k p) n -> p k n", p=128), ())]).rearrange("p (k n) -> p k n", k=KQ)
                    CP(eselc[0:32, :], ident[0:32, e:e + 1].to_broadcast([32, 128]))
                    for fc in range(2):
                        for ci, (c0, cn) in enumerate(CT):
                            pu = nb(); pg = nb(); pc = nb()
                            for k in range(KQ):
                                MM(pu[:, 0:cn], wv[:, k, fc * 128:(fc + 1) * 128], hT[:, k, c0:c0 + cn], k == 0, k == KQ - 1)
                            for k in range(KQ):
                                MM(pg[:, 0:cn], wv[:, k, 256 + fc * 128:256 + (fc + 1) * 128], hT[:, k, c0:c0 + cn], k == 0, k == KQ - 1)
                            MM(pc[:, 0:cn], eselc[0:32, :], combT[0:32, c0:c0 + cn], True, True)
                            ACT(tA[:, 0:cn], pg[:, 0:cn], AF.Silu)
                            TT(tA[:, 0:cn], tA[:, 0:cn], pu[:, 0:cn], ALU.mult)
                            TT(act[:, el * 2 + fc, c0:c0 + cn], tA[:, 0:cn], pc[:, 0:cn], ALU.mult)
                for mb in range(4):
                    src = w_dn[l, ep * EP:(ep + 1) * EP, :, mb * 512:(mb + 1) * 512].rearrange("e (f p) n -> p (e f) n", p=128)
                    wv = wget([(lambda s: s.rearrange("p (k n) -> p k n", k=KQ)[:, 0:EP * 2, :], src, ())]).rearrange("p (k n) -> p k n", k=KQ)
                    for m_ in range(4):
                        mch = mb * 4 + m_
                        for ci, (c0, cn) in enumerate(CT):
                            pb = nb()
                            for a in range(EP * 2):
                                MM(pb[:, 0:cn], wv[:, a, m_ * 128:(m_ + 1) * 128], act[:, a, c0:c0 + cn], a == 0, a == EP * 2 - 1)
                            resid_add(mch, ci, pb, 5, tmpB)
            cur[0] = o_r + KQ * NT
            L = ln_alloc()
            ln_stats(L, lambda k: rbuf[:, k, 0:NTc], KQ, LN_EPS)
            ln_affine_inplace(L, 32)
            if l == 0: dbg("x2", rbuf[:, :, 0:NTc])
            cur[0] = o_r + KQ * NT

        cur[0] = o_r + KQ * NT
        yt = [f32v(alloc(D), D), f32v(alloc(D), D)]
        yseg = yp_o[seg * NP:(seg + 1) * NP, :].rearrange("(c j) d -> j c d", j=8)
        for j in range(9 if isA else 8):
            t = yt[j % 2]
            for g4 in range(4):
                pb = nb()
                for q in range(4):
                    kq = g4 * 4 + q
                    TR(pb[:, q * 128:(q + 1) * 128], rbuf[:, kq, j * 128:(j + 1) * 128], ident)
                CP(t[:, g4 * 512:(g4 + 1) * 512], pb[:, 0:512])
            if j < 8:
                DMA(yseg[j], t)
            else:
                for jj in range(8):
                    DMA(ys_o[:, jj, :], t[jj * 16:(jj + 1) * 16, :])
        cur[0] = p0

    def ssm_prep(l):
        c0_ = cur[0]
        o_apw = alloc(640)
        a8t = f32v(alloc(64), 64)
        c_l0 = cur[0]
        o_l0 = alloc(128 * 26)

        def L0(i): return f32v(o_l0 + i * 128, 128)[0:32, :]
        are, aim, mag, ang, t0, t1, cosv, sinv, kre, kim, den = [L0(i) for i in range(11)]
        pw_re = [L0(11 + i) for i in range(7)]; pw_im = [L0(18 + i) for i in range(7)]
        dtt = f32v(alloc(2), 2)[0:32, :]
        v3 = lambda a: a.rearrange("g (h p) -> g h p", h=2)
        DMA(v3(are), a_re_d[l].rearrange("(h g) p -> g h p", h=2))
        DMA(v3(aim), a_im_d[l].rearrange("(h g) p -> g h p", h=2))
        for h in range(2):
            DMA(dtt[:, h:h + 1], ldt_d[l][h * 32:(h + 1) * 32].rearrange("(g o) -> g o", o=1))
        ACT(dtt, dtt, AF.Exp)
        dtb = dtt.unsqueeze(2).to_broadcast([32, 2, 64])
        TT(v3(t0), v3(are), dtb, ALU.mult)
        ACT(mag, t0, AF.Exp)
        TT(v3(ang), v3(aim), dtb, ALU.mult)
        TWO_PI = 2.0 * math.pi; MAGIC = 12582912.0

        def sin_of(dst, shift):
            TS(t0, ang, shift, 1.0 / TWO_PI, ALU.add, ALU.mult)
            TS(t1, t0, MAGIC, None, ALU.add)
            TS(t1, t1, -MAGIC, None, ALU.add)
            TT(t0, t0, t1, ALU.subtract)
            ACT(dst, t0, AF.Sin, scale=TWO_PI)
        sin_of(sinv, 0.0)
        sin_of(cosv, math.pi / 2.0)
        ab_re, ab_im = cosv, sinv
        TT(ab_re, cosv, mag, ALU.mult)
        TT(ab_im, sinv, mag, ALU.mult)
        TT(den, are, are, ALU.mult)
        TT(t0, aim, aim, ALU.mult)
        TT(den, den, t0, ALU.add)
        RCP(den, den)
        TS(t1, ab_re, -1.0, None, ALU.add)
        TT(kre, t1, are, ALU.mult)
        TT(t0, ab_im, aim, ALU.mult)
        TT(kre, kre, t0, ALU.add)
        TT(kre, kre, den, ALU.mult)
        TT(kim, ab_im, are, ALU.mult)
        TT(t0, t1, aim, ALU.mult)
        TT(kim, kim, t0, ALU.subtract)
        TT(kim, kim, den, ALU.mult)
        prev_re, prev_im = ab_re, ab_im
        for i in range(7):
            nr, ni = pw_re[i], pw_im[i]
            TT(nr, prev_re, ab_re, ALU.mult)
            TT(t0, prev_im, ab_im, ALU.mult)
            TT(nr, nr, t0, ALU.subtract)
            TT(ni, prev_re, ab_im, ALU.mult)
            TT(t1, prev_im, ab_re, ALU.mult)
            TT(ni, ni, t1, ALU.add)
            prev_re, prev_im = nr, ni
        APW = f32v(o_apw, 576).rearrange("p (m r g) -> p m r g", m=9, r=2)
        KK = f32v(o_apw + 576, 64).rearrange("p (r g) -> p r g", r=2)
        MS(APW[:, 0, 0, :], 1.0); MS(APW[:, 0, 1, :], 0.0)
        srcs = [(ab_re, APW[:, 1, 0, :]), (ab_im, APW[:, 1, 1, :])]
        for i in range(7):
            srcs.append((pw_re[i], APW[:, i + 2, 0, :])); srcs.append((pw_im[i], APW[:, i + 2, 1, :]))
        srcs.append((kre, KK[:, 0, :])); srcs.append((kim, KK[:, 1, :]))
        for (s_ap, d_ap) in srcs:
            pb = nb()
            TR(pb[:, 0:32], s_ap, ident[0:32, 0:32])
            CP(d_ap, pb[:, 0:32])
        CP(a8t[:, 0:32], APW[:, 8, 0, :]); CP(a8t[:, 32:64], APW[:, 8, 1, :])
        DMA(A8_scr[l], a8t, w=[("A8_scr", l)])
        cur[0] = c_l0
        Bt = f32v(alloc(1024), 1024).rearrange("p (r g k) -> p r g k", r=2, g=32)
        for ri_, bd in enumerate([b_re_d, b_im_d]):
            for gh in range(2):
                DMA(Bt[gh * 64:(gh + 1) * 64, ri_, :, :], bd[l][gh * 32:(gh + 1) * 32].rearrange("g p k -> p g k"))
        Bb = f32v(alloc(1024), 1024).rearrange("p (r g k) -> p r g k", r=2, g=32)
        t5 = f32v(alloc(512), 512).rearrange("p (g k) -> p g k", g=32)
        t6 = f32v(alloc(512), 512).rearrange("p (g k) -> p g k", g=32)
        bc = lambda a: a.unsqueeze(2).to_broadcast([128, 32, 16])
        TT(Bb[:, 0], Bt[:, 0], bc(KK[:, 0, :]), ALU.mult)
        TT(t5, Bt[:, 1], bc(KK[:, 1, :]), ALU.mult)
        TT(Bb[:, 0], Bb[:, 0], t5, ALU.subtract)
        TT(Bb[:, 1], Bt[:, 1], bc(KK[:, 0, :]), ALU.mult)
        TT(t5, Bt[:, 0], bc(KK[:, 1, :]), ALU.mult)
        TT(Bb[:, 1], Bb[:, 1], t5, ALU.add)
        Er = f32v(alloc(8192), 8192).rearrange("p (r g j k) -> p r g j k", r=2, g=32, j=8)
        for j in range(8):
            m = 7 - j
            TT(Er[:, 0, :, j, :], Bb[:, 0], bc(APW[:, m, 0, :]), ALU.mult)
            TT(t5, Bb[:, 1], bc(APW[:, m, 1, :]), ALU.mult)
            TT(Er[:, 0, :, j, :], Er[:, 0, :, j, :], t5, ALU.subtract)
            TT(Er[:, 1, :, j, :], Bb[:, 1], bc(APW[:, m, 0, :]), ALU.mult)
            TT(t5, Bb[:, 0], bc(APW[:, m, 1, :]), ALU.mult)
            TT(Er[:, 1, :, j, :], Er[:, 1, :, j, :], t5, ALU.add)
        c1_ = cur[0]
        Rm = bf16v(alloc(4096), 8192).rearrange("p (g r q) -> p g r q", g=64, r=2)
        for gg in range(32):
            pb = nb()
            for ri_ in range(2):
                TR(pb[:, ri_ * 128:(ri_ + 1) * 128], Er[:, ri_, gg].rearrange("p j k -> p (j k)"), ident)
            for gh in range(2):
                CP(Rm[:, gh * 32 + gg, :, :], pb[:, 0:256].rearrange("p (r h q) -> p r h q", r=2, h=2)[:, :, gh, :])
        DMA(R_scr[l], Rm.rearrange("p g r q -> p (g r q)"), w=[("R_scr", l)])
        cur[0] = c1_
        CTt = f32v(alloc(1024), 1024).rearrange("p (r g k) -> p r g k", r=2, g=32)
        cst = f32v(alloc(128), 128)
        for ri_, cd in enumerate([c_re_d, c_im_d]):
            for g8 in range(4):
                for gh in range(2):
                    DMA(cst[:, gh * 64:(gh + 1) * 64], cd[l][gh * 32 + g8 * 8: gh * 32 + g8 * 8 + 8].rearrange("g k p -> (g k) p"))
                pb = nb()
                TR(pb[:, 0:128], cst, ident)
                CP(CTt[:, ri_, g8 * 8:(g8 + 1) * 8, :], pb[:, 0:128].rearrange("p (g k) -> p g k", g=8))
        nCi = f32v(alloc(512), 512).rearrange("p (g k) -> p g k", g=32)
        TS(nCi, CTt[:, 1], -1.0, None, ALU.mult)
        Kst = f32v(alloc(1024), 1024)
        for half in range(2):
            pb = nb()
            for gl in range(32):
                g = half * 32 + gl; gh = g // 32; gg = g % 32
                sl = slice(gh * 64, (gh + 1) * 64)
                MM(pb[:, gl * 16:(gl + 1) * 16], Er[sl, 0, gg].rearrange("p j k -> p (j k)"), CTt[sl, 0, gg, :], True, False)
                MM(pb[:, gl * 16:(gl + 1) * 16], Er[sl, 1, gg].rearrange("p j k -> p (j k)"), nCi[sl, gg, :], False, True)
            CP(Kst[:, half * 512:(half + 1) * 512], pb[:, 0:512])
        drep = f32v(alloc(64), 64)
        dsel = f32v(alloc(1024), 1024).rearrange("p (a m) -> p a m", a=8)
        for a in range(8):
            CP(dsel[:, a, :].rearrange("p (j k) -> p j k", j=8), ident[:, a * 16:(a + 1) * 16].unsqueeze(1).to_broadcast([128, 8, 16]))
        pb = nb()
        for a in range(8):
            MM(pb[:, a * 8:(a + 1) * 8], dsel[:, a, :], pv2[:, 56:64], True, True)
        CP(drep.rearrange("p (q a) -> p q a", q=8), pb[:, 0:64].rearrange("p (a q) -> p q a", a=8))
        Tm = bf16v(alloc(4096), 8192).rearrange("p (g n) -> p g n", g=64)
        Tf = f32v(alloc(4096), 4096).rearrange("p (g n) -> p g n", g=32)
        for half in range(2):
            for i in range(8):
                pb = nb()
                MM(pb[:, 0:512], shm[:, i, :], Kst[:, half * 512:(half + 1) * 512], True, True)
                CP(Tf[:, :, i * 16:(i + 1) * 16], pb[:, 0:512].rearrange("p (g k) -> p g k", g=32))
            for gl in range(32):
                g = half * 32 + gl
                STT(Tm[:, g, :], ident, drep[:, g:g + 1], Tf[:, gl, :], ALU.mult, ALU.add)
        DMA(T_scr[l], Tm.rearrange("p g n -> p (g n)"), w=[("T_scr", l)])
        Om = Tf.rearrange("p g n -> p (g n)").rearrange("p (g n) -> p g n", g=32)
        for ri_ in range(2):
            for i in range(8):
                ar_b = bc(APW[:, i + 1, 0, :]); ai_b = bc(APW[:, i + 1, 1, :])
                osl = Om[:, :, i * 16:(i + 1) * 16]
                if ri_ == 0:
                    TT(t5, CTt[:, 0], ar_b, ALU.mult)
                    TT(t6, CTt[:, 1], ai_b, ALU.mult)
                    TT(osl, t5, t6, ALU.subtract)
                else:
                    TT(t5, CTt[:, 0], ai_b, ALU.mult)
                    TT(t6, nCi, ar_b, ALU.mult)
                    TT(osl, t6, t5, ALU.subtract)
            DMA(O_scr[l][ri_], Om.rearrange("p g n -> p (g n)"), w=[("O_scr", l)])
        cur[0] = c0_

    P.dry = True
    body()
    P.dry = False
    P.ops = []
    wstate["i"] = 0; wstate["issued"] = 0; wstate["rec"] = False
    body()
    P.emit()
    return nc, es, P


_SHM = None


def _consts():
    global _SHM
    if _SHM is None:
        shm = np.zeros((8, 128, 128), np.float32)
        for i in range(8):
            for j in range(i + 1):
                s = 7 - i + j
                for k in range(16):
                    shm[i, s * 16 + k, j * 16 + k] = 1.0
        _SHM = shm
    return np.eye(128, dtype=np.float32), _SHM


_WNAMES = ["w_ada", "b_ada", "w_in", "ssm_a_re", "ssm_a_im", "ssm_log_dt", "ssm_b_re", "ssm_b_im", "ssm_c_re", "ssm_c_im",
           "ssm_d", "w_s5_val", "w_s5_gate", "conv_w", "conv_b", "conv_ln_g", "conv_ln_b", "w_conv_pw", "w_out",
           "ln1_g", "ln1_b", "moe_w_group", "moe_b_group", "moe_w_router", "moe_b_router",
           "moe_w_up", "moe_w_gate", "moe_w_down", "ln2_g", "ln2_b"]


def make_in_maps(inputs):
    ident, shm = _consts()
    f = lambda a: np.ascontiguousarray(np.asarray(a, dtype=np.float32))
    shared = {n: f(inputs[n]) for n in _WNAMES}
    shared["ident"] = ident; shared["shm"] = shm
    xp = f(inputs["x_prompt"]); xs = f(inputs["x_sample"]); cp = f(inputs["c_prompt"]); cs = f(inputs["c_sample"])
    sre = f(inputs["state_ssm_re"]); sim = f(inputs["state_ssm_im"]); scv = f(inputs["state_conv"])
    maps = []
    for c in range(NCORES):
        b = c % 4
        m = dict(shared)
        m["xp"] = xp[b]
        m["xs"] = np.ascontiguousarray(xs[16 * c:16 * c + 16])
        m["cvec"] = np.ascontiguousarray(np.concatenate([cp[b:b + 1], cs[16 * c:16 * c + 16]], axis=0))
        m["sre"] = np.ascontiguousarray(sre[:, 16 * c:16 * c + 16])
        m["sim"] = np.ascontiguousarray(sim[:, 16 * c:16 * c + 16])
        m["scv"] = np.ascontiguousarray(scv[:, 16 * c:16 * c + 16])
        maps.append(m)
    return maps


_PROG = None


def kernel(**inputs):
    global _PROG
    if _PROG is None:
        _PROG = build_program()
    nc = _PROG[0]
    maps = make_in_maps(inputs)
    res = run_bass_kernel_spmd(nc, maps, core_ids=list(range(NCORES)))
    R = res.results
    y_prompt = np.stack([R[b]["yp"] for b in range(4)], axis=0)
    y_sample = np.concatenate([R[c]["ys"] for c in range(NCORES)], axis=0)
    p_re = np.stack([R[b]["pre"] for b in range(4)], axis=1)
    p_im = np.stack([R[b]["pim"] for b in range(4)], axis=1)
    p_cv = np.stack([R[b]["pcv"] for b in range(4)], axis=1)
    s_re = np.concatenate([R[c]["sre_o"] for c in range(NCORES)], axis=1)
    s_im = np.concatenate([R[c]["sim_o"] for c in range(NCORES)], axis=1)
    s_cv = np.concatenate([R[c]["scv_o"] for c in range(NCORES)], axis=1)
    f = lambda a: np.ascontiguousarray(a, dtype=np.float32)
    return (f(y_prompt), f(y_sample), f(p_re), f(p_im), f(p_cv), f(s_re), f(s_im), f(s_cv))
```

```python
import math
import numpy as np
from contextlib import ExitStack
import concourse.bass as bass
import concourse.mybir as mybir
from concourse.bass_utils import run_bass_kernel_spmd

F32 = mybir.dt.float32
BF16 = mybir.dt.bfloat16
AF = mybir.ActivationFunctionType
ALU = mybir.AluOpType

D = 2048; DEPTH = 4; NT = 1152; NP = 1024; KQ = 16
ALPHA = (2 * DEPTH) ** 0.25
LN_EPS = 1e-5
NCORES = 8
ARENA = 53200
BLK = 256


def _ap_keys(ap):
    sp = str(ap.space).upper()
    if not ("SB" in sp or "PSUM" in sp):
        return None
    es = 2 if "bfloat16" in str(ap.dtype) else 4
    dims = list(ap.ap)
    pstep = dims[0][0]
    off = ap.offset % pstep if pstep > 0 else ap.offset
    ext = 1
    for (st, cnt) in dims[1:]:
        ext += abs(st) * (cnt - 1)
    lo = off * es; hi = (off + ext) * es
    name = ap.tensor.name
    return [(name, b) for b in range(lo // BLK, (hi - 1) // BLK + 1)]


class Prog:
    COMPUTE = ("pe", "act", "dve", "pool")

    def __init__(self, nc, es, n_dma_sems=28, sem_cap=8000):
        self.nc = nc; self.es = es; self.ops = []
        self.eng = {"pe": nc.tensor, "act": nc.scalar, "dve": nc.vector, "pool": nc.gpsimd, "sp": nc.sync}
        self.n_dma_sems = n_dma_sems; self.sem_cap = sem_cap

    def op(self, eng, fn, ins=(), outs=(), r=(), w=(), dma=False):
        if getattr(self, "dry", False):
            return
        rk = list(r); wk = list(w)
        for a in ins:
            k = _ap_keys(a)
            if k: rk += k
        for a in outs:
            k = _ap_keys(a)
            if k: wk += k
        self.ops.append(dict(eng=eng, fn=fn, r=rk, w=wk, dma=dma, deps=None, need=False))

    def emit(self):
        nc, es, ops = self.nc, self.es, self.ops
        lastw = {}; readers = {}
        for i, o in enumerate(ops):
            deps = set()
            for k in o["r"]:
                d = lastw.get(k)
                if d is not None: deps.add(d)
            for k in o["w"]:
                d = lastw.get(k)
                if d is not None: deps.add(d)
                rd = readers.get(k)
                if rd:
                    deps.update(rd.values())
            deps.discard(i)
            if o["eng"] == "pe" and not o["dma"]:
                deps = {d for d in deps if not (ops[d]["eng"] == "pe" and not ops[d]["dma"])}
            o["deps"] = deps
            for d in deps: ops[d]["need"] = True
            ek = ("dma", i) if o["dma"] else o["eng"]
            for k in o["r"]:
                rd = readers.get(k)
                if rd is None: readers[k] = {ek: i}
                else: rd[ek] = i
            for k in o["w"]:
                lastw[k] = i; readers[k] = None
            o["r"] = None; o["w"] = None
        cnt = {e: 0 for e in self.COMPUTE}; epoch = {e: 0 for e in self.COMPUTE}
        sems = {e: [es.enter_context(nc.semaphore(f"s_{e}_0"))] for e in self.COMPUTE}
        dsems = [es.enter_context(nc.semaphore(f"s_dma_{i}")) for i in range(self.n_dma_sems)]
        dval = [0] * self.n_dma_sems; dnext = 0
        for o in ops:
            if o["dma"]:
                o["dsem"] = dnext; dnext = (dnext + 1) % self.n_dma_sems
            elif o["need"]:
                e = o["eng"]
                if cnt[e] >= self.sem_cap:
                    epoch[e] += 1; cnt[e] = 0
                    sems[e].append(es.enter_context(nc.semaphore(f"s_{e}_{epoch[e]}")))
                cnt[e] += 1
                o["sem"] = (e, epoch[e], cnt[e])
        waited = {}
        for o in ops:
            weng = o["eng"]
            for d in sorted(o["deps"]):
                od = ops[d]
                if od["dma"]:
                    key = (weng, ("d", od["dsem"]))
                    if waited.get(key, -1) >= od["dval"]: continue
                    waited[key] = od["dval"]
                    self.eng[weng].wait_ge(dsems[od["dsem"]], od["dval"])
                else:
                    e, ep, v = od["sem"]
                    key = (weng, ("c", e))
                    if waited.get(key, (-1, -1)) >= (ep, v): continue
                    waited[key] = (ep, v)
                    self.eng[weng].wait_ge(sems[e][ep], v)
            if o["dma"]:
                si = o["dsem"]
                if dval[si] > 0:
                    key = (weng, ("d", si))
                    if waited.get(key, -1) < dval[si]:
                        waited[key] = dval[si]
                        self.eng[weng].wait_ge(dsems[si], dval[si])
                ins = o["fn"]()
                dval[si] += 16; o["dval"] = dval[si]
                ins.then_inc(dsems[si], 16)
            else:
                ins = o["fn"]()
                if o["need"]:
                    e, ep, v = o["sem"]
                    ins.then_inc(sems[e][ep], 1)
            o["fn"] = None
        for si in range(self.n_dma_sems):
            if dval[si] > 0:
                nc.sync.wait_ge(dsems[si], dval[si])
        self.stats = dict(n_ops=len(ops), incs={e: (epoch[e], cnt[e]) for e in self.COMPUTE})


def build_program(depth=DEPTH, passes=("A", "B"), debug=None, stop_after=None):
    nc = bass.Bass("TRN2", target_bir_lowering=False)
    es = ExitStack()
    P = Prog(nc, es)

    def din(name, shape):
        return nc.dram_tensor(name, list(shape), F32, kind="ExternalInput").ap()

    def dout(name, shape):
        return nc.dram_tensor(name, list(shape), F32, kind="ExternalOutput").ap()

    def dscr(name, shape, dt=F32):
        return nc.dram_tensor(name, list(shape), dt, kind="Internal").ap()

    xp = din("xp", [2 * NP, D]); xs = din("xs", [16, 8, D]); cvec = din("cvec", [17, D])
    sre_i = din("sre", [DEPTH, 16, 64, 64]); sim_i = din("sim", [DEPTH, 16, 64, 64])
    scv_i = din("scv", [DEPTH, 16, 30, 1024])
    ident_d = din("ident", [128, 128]); shm_d = din("shm", [8, 128, 128])
    w_ada = din("w_ada", [DEPTH, D, 6 * D]); b_ada = din("b_ada", [DEPTH, 6 * D])
    w_in = din("w_in", [DEPTH, D, 7168])
    a_re_d = din("ssm_a_re", [DEPTH, 64, 64]); a_im_d = din("ssm_a_im", [DEPTH, 64, 64])
    ldt_d = din("ssm_log_dt", [DEPTH, 64])
    b_re_d = din("ssm_b_re", [DEPTH, 64, 64, 16]); b_im_d = din("ssm_b_im", [DEPTH, 64, 64, 16])
    c_re_d = din("ssm_c_re", [DEPTH, 64, 16, 64]); c_im_d = din("ssm_c_im", [DEPTH, 64, 16, 64])
    ssm_d_d = din("ssm_d", [DEPTH, 1024])
    w_val = din("w_s5_val", [DEPTH, 1024, D]); w_gate = din("w_s5_gate", [DEPTH, 1024, D])
    conv_w_d = din("conv_w", [DEPTH, 31, 1024]); conv_b_d = din("conv_b", [DEPTH, 1024])
    cln_g_d = din("conv_ln_g", [DEPTH, 1024]); cln_b_d = din("conv_ln_b", [DEPTH, 1024])
    w_pw = din("w_conv_pw", [DEPTH, 1024, D]); w_out = din("w_out", [DEPTH, D, D])
    ln1_g_d = din("ln1_g", [DEPTH, D]); ln1_b_d = din("ln1_b", [DEPTH, D])
    mwg_d = din("moe_w_group", [DEPTH, D, 4]); mbg_d = din("moe_b_group", [DEPTH, 4])
    mwr_d = din("moe_w_router", [DEPTH, D, 32]); mbr_d = din("moe_b_router", [DEPTH, 32])
    w_up = din("moe_w_up", [DEPTH, 32, D, 256]); w_gt = din("moe_w_gate", [DEPTH, 32, D, 256])
    w_dn = din("moe_w_down", [DEPTH, 32, 256, D])
    ln2_g_d = din("ln2_g", [DEPTH, D]); ln2_b_d = din("ln2_b", [DEPTH, D])

    yp_o = dout("yp", [2 * NP, D]); ys_o = dout("ys", [16, 8, D])
    pre_o = dout("pre", [DEPTH, 64, 64]); pim_o = dout("pim", [DEPTH, 64, 64])
    pcv_o = dout("pcv", [DEPTH, 30, 1024])
    sre_o = dout("sre_o", [DEPTH, 16, 64, 64]); sim_o = dout("sim_o", [DEPTH, 16, 64, 64])
    scv_o = dout("scv_o", [DEPTH, 16, 30, 1024])
    dbg_o = {}
    if debug:
        for k, (shp, dtn) in debug.items():
            dbg_o[k] = nc.dram_tensor("dbg_" + k, list(shp), BF16 if dtn == "bf16" else F32, kind="ExternalOutput").ap()

    xscr = dscr("xscr", [KQ, 128, NT])
    ada_scr = dscr("ada_scr", [DEPTH, 128, 96 * 17])
    R_scr = dscr("R_scr", [DEPTH, 128, 8192], BF16)
    T_scr = dscr("T_scr", [DEPTH, 128, 8192], BF16)
    O_scr = dscr("O_scr", [DEPTH, 2, 128, 4096])
    A8_scr = dscr("A8_scr", [DEPTH, 128, 64])

    arena = es.enter_context(nc.sbuf_tensor("arena", [128, ARENA], F32))
    cur = [0]

    def alloc(nwords):
        if nwords >= 256:
            cur[0] = (cur[0] + 63) // 64 * 64
        o = cur[0]; cur[0] += nwords
        assert cur[0] <= ARENA, ("SBUF arena overflow", cur[0], ARENA)
        return o

    def f32v(off, n):
        return arena[:, off:off + n]

    def bf16v(off, nbf):
        assert nbf % 2 == 0
        return arena[:, off:off + nbf // 2].bitcast(BF16)

    ps = [es.enter_context(nc.psum_tensor(f"ps{i}", [128, 512], F32)) for i in range(8)]
    psn = [0]

    def nb():
        i = psn[0] % 8; psn[0] += 1
        return ps[i]

    def MM(out, lhsT, rhs, start, stop):
        P.op("pe", lambda: nc.tensor.matmul(out, lhsT=lhsT, rhs=rhs, start=start, stop=stop), ins=[lhsT, rhs], outs=[out])

    def TR(out, in_, idn):
        P.op("pe", lambda: nc.tensor.transpose(out, in_, idn), ins=[in_, idn], outs=[out])

    def ACT(out, in_, func, scale=1.0, bias=0.0):
        ins = [in_] + [a for a in (scale, bias) if not isinstance(a, (int, float))]
        P.op("act", lambda: nc.scalar.activation(out=out, in_=in_, func=func, scale=scale, bias=bias), ins=ins, outs=[out])

    def TT(out, in0, in1, op):
        P.op("dve", lambda: nc.vector.tensor_tensor(out=out, in0=in0, in1=in1, op=op), ins=[in0, in1], outs=[out])

    def TS(out, in0, s1, s2, op0, op1=None):
        ins = [in0] + [a for a in (s1, s2) if a is not None and not isinstance(a, (int, float))]
        if s2 is None:
            P.op("dve", lambda: nc.vector.tensor_scalar(out=out, in0=in0, scalar1=s1, scalar2=None, op0=op0), ins=ins, outs=[out])
        else:
            P.op("dve", lambda: nc.vector.tensor_scalar(out=out, in0=in0, scalar1=s1, scalar2=s2, op0=op0, op1=op1), ins=ins, outs=[out])

    def STT(out, in0, scalar, in1, op0, op1):
        ins = [in0, in1] + ([] if isinstance(scalar, (int, float)) else [scalar])
        P.op("dve", lambda: nc.vector.scalar_tensor_tensor(out=out, in0=in0, scalar=scalar, in1=in1, op0=op0, op1=op1), ins=ins, outs=[out])

    def TTP(out, in0, in1, op):
        P.op("pool", lambda: nc.gpsimd.tensor_tensor(out=out, in0=in0, in1=in1, op=op), ins=[in0, in1], outs=[out])

    def ACP(out, in_):
        P.op("act", lambda: nc.scalar.activation(out=out, in_=in_, func=AF.Copy), ins=[in_], outs=[out])

    def CP(out, in_):
        P.op("dve", lambda: nc.vector.tensor_copy(out=out, in_=in_), ins=[in_], outs=[out])

    def MS(ap, val):
        P.op("dve", lambda: nc.vector.memset(ap, val), outs=[ap])

    def RCP(out, in_):
        P.op("dve", lambda: nc.vector.reciprocal(out=out, in_=in_), ins=[in_], outs=[out])

    def DMA(out, in_, r=(), w=(), q="sp"):
        e = {"sp": nc.sync, "pool": nc.gpsimd, "act": nc.scalar}[q]
        P.op(q, lambda: e.dma_start(out=out, in_=in_), ins=[in_], outs=[out], r=r, w=w, dma=True)

    ident = f32v(alloc(128), 128)
    identb = bf16v(alloc(64), 128)
    onesb = bf16v(alloc(64), 128)
    shm = f32v(alloc(1024), 1024).rearrange("p (i m) -> p i m", i=8)
    scT = bf16v(alloc(KQ * 18 // 2), KQ * 18).rearrange("p (k b) -> p k b", k=KQ)[:, :, 0:17]
    adaT = f32v(alloc(96 * 17), 96 * 17).rearrange("p (c b) -> p c b", c=96)
    pv1 = f32v(alloc(128), 128)
    pv2 = f32v(alloc(64), 64)
    cw = f32v(alloc(248), 248)
    gA = f32v(alloc(64), 64)
    rbias = f32v(alloc(36), 36)
    badaN = f32v(alloc(96), 96)
    astage = [f32v(alloc(68), 68) for _ in range(2)]
    A8 = f32v(alloc(64), 64)
    stsave = f32v(alloc(DEPTH * 64), DEPTH * 64).rearrange("p (l r g) -> p l r g", l=DEPTH, r=2)
    cvsave = f32v(alloc(DEPTH * 240), DEPTH * 240).rearrange("p (l q t) -> p l q t", l=DEPTH, q=8)
    hT = bf16v(alloc(KQ * NT // 2), KQ * NT).rearrange("p (k n) -> p k n", k=KQ)
    NWB = 3
    wb = [bf16v(alloc(4096), 8192) for _ in range(NWB)]
    wb3 = [w_.rearrange("p (k n) -> p k n", k=KQ) for w_ in wb]
    base_phase = cur[0]

    wseq = []
    wstate = {"i": 0, "issued": 0, "rec": True}
    PREF = 2

    def wget(parts, keep=False):
        i = wstate["i"]; wstate["i"] += 1
        if wstate["rec"]:
            wseq.append(parts)
            return wb[i % NWB]
        live = wstate.setdefault("live", set())
        if not keep:
            live.clear()
        live.add(i)
        while wstate["issued"] < min(len(wseq), i + 1 + PREF):
            j = wstate["issued"]
            if any((x % NWB) == (j % NWB) and x != j for x in live):
                break
            wstate["issued"] += 1
            for (dst_fn, src, rkeys) in wseq[j]:
                DMA(dst_fn(wb[j % NWB]), src, r=rkeys, q="pool")
        assert wstate["issued"] > i, "weight slot conflict: too many live items"
        return wb[i % NWB]

    def wpart(mat, r0, nk, c0, ncol, k0=0, cc0=0):
        src = mat[r0:r0 + nk * 128, c0:c0 + ncol].rearrange("(k p) n -> p k n", p=128)
        return (lambda s: s.rearrange("p (k n) -> p k n", k=KQ)[:, k0:k0 + nk, cc0:cc0 + ncol], src, ())

    def body():
        cur[0] = base_phase
        psn[0] = 0
        DMA(ident, ident_d[:, :])
        DMA(shm, shm_d.rearrange("i p m -> p i m"))
        CP(identb, ident)
        MS(onesb, 1.0)
        o_c = alloc(D); ctile = f32v(o_c, D)
        DMA(ctile[0:17, :], cvec[:, :])
        ACT(ctile[0:17, :], ctile[0:17, :], AF.Silu)
        for g4 in range(4):
            pb = nb()
            for q in range(4):
                kq = g4 * 4 + q
                TR(pb[:, q * 17:(q + 1) * 17], ctile[0:17, kq * 128:(kq + 1) * 128], ident[0:17, 0:17])
            CP(scT[:, g4 * 4:(g4 + 1) * 4, :], pb[:, 0:68].rearrange("p (a b) -> p a b", a=4))
        cur[0] = o_c

        for pas in passes:
            run_pass(pas)

    def run_pass(pas):
        isA = (pas == "A")
        NTc = NT if isA else NP
        CT = [(0, 512), (512, 512)] + ([(1024, 128)] if isA else [])
        seg = 0 if isA else 1
        p0 = cur[0]

        def dbg(name, ap_sb):
            if debug and (pas + "_" + name) in dbg_o:
                DMA(dbg_o[pas + "_" + name], ap_sb)

        o_r = alloc(KQ * NT); rbuf = f32v(o_r, KQ * NT).rearrange("p (k n) -> p k n", k=KQ)
        o_xt = alloc(2 * D); xt = [f32v(o_xt, D), f32v(o_xt + D, D)]
        xseg = xp[seg * NP:(seg + 1) * NP, :].rearrange("(c j) d -> j c d", j=8)
        for j in range(9 if isA else 8):
            t = xt[j % 2]
            if j < 8:
                DMA(t, xseg[j])
            else:
                for jj in range(8):
                    DMA(t[jj * 16:(jj + 1) * 16, :], xs[:, jj, :])
            for g4 in range(4):
                pb = nb()
                for q in range(4):
                    kq = g4 * 4 + q
                    TR(pb[:, q * 128:(q + 1) * 128], t[:, kq * 128:(kq + 1) * 128], ident)
                ACT(rbuf[:, g4 * 4:(g4 + 1) * 4, j * 128:(j + 1) * 128], pb.rearrange("p (a b) -> p a b", a=4), AF.Copy, scale=ALPHA)
        cur[0] = o_xt

        def ln_alloc():
            d = {}
            d["mean"] = f32v(alloc(NT), NT); d["rstd"] = f32v(alloc(NT), NT)
            d["tmpA"] = f32v(alloc(NT), NT); d["tmpB"] = f32v(alloc(128), 128)
            d["tmpA2"] = [d["tmpA"], f32v(alloc(NT), NT)]
            d["tb"] = []
            for _ in range(2):
                o = alloc(NT); d["tb"].append((bf16v(o, NT), bf16v(o + NT // 2, NT)))
            return d

        def ln_stats(L, src_fn, nch, eps):
            banks = [nb() for _ in range(2 * len(CT))]
            for k in range(nch):
                tb1, tb2 = L["tb"][k % 2]
                CP(tb1[:, 0:NTc], src_fn(k))
                ACT(tb2[:, 0:NTc], src_fn(k), AF.Square)
                for ci, (c0, cn) in enumerate(CT):
                    MM(banks[ci][:, 0:cn], onesb, tb1[:, c0:c0 + cn], k == 0, k == nch - 1)
                    MM(banks[len(CT) + ci][:, 0:cn], onesb, tb2[:, c0:c0 + cn], k == 0, k == nch - 1)
            nf = float(nch * 128)
            for ci, (c0, cn) in enumerate(CT):
                TS(L["mean"][:, c0:c0 + cn], banks[ci][:, 0:cn], 1.0 / nf, None, ALU.mult)
                TS(L["rstd"][:, c0:c0 + cn], banks[len(CT) + ci][:, 0:cn], 1.0 / nf, None, ALU.mult)
            m = L["mean"][:, 0:NTc]; r_ = L["rstd"][:, 0:NTc]; ta = L["tmpA"][:, 0:NTc]
            TT(ta, m, m, ALU.mult)
            TT(r_, r_, ta, ALU.subtract)
            TS(r_, r_, eps, None, ALU.add)
            ACT(r_, r_, AF.Sqrt)
            RCP(r_, r_)

        def modulate_to_h(L, part_sc, part_sh):
            m = L["mean"][:, 0:NTc]; r_ = L["rstd"][:, 0:NTc]
            for k in range(KQ):
                ta = L["tmpA2"][k % 2]
                (TTP if k % 2 == 0 else TT)(ta[:, 0:NTc], rbuf[:, k, 0:NTc], m, ALU.subtract)
                TT(ta[:, 0:NTc], ta[:, 0:NTc], r_, ALU.mult)
                sc = adaT[:, part_sc * 16 + k, :]; sh = adaT[:, part_sh * 16 + k, :]
                ACT(hT[:, k, 0:NP], ta[:, 0:NP], AF.Identity, scale=sc[:, 0:1], bias=sh[:, 0:1])
                if isA:
                    t3 = ta[:, NP:NT].rearrange("p (j s) -> p j s", j=8)
                    TT(t3, t3, sc[:, 1:17].unsqueeze(1).to_broadcast([128, 8, 16]), ALU.mult)
                    TT(hT[:, k, NP:NT].rearrange("p (j s) -> p j s", j=8), t3, sh[:, 1:17].unsqueeze(1).to_broadcast([128, 8, 16]), ALU.add)

        def resid_add(k, ci, pb, gpart, tmpB):
            c0, cn = CT[ci]
            g = adaT[:, gpart * 16 + k, :]
            if ci < 2:
                STT(rbuf[:, k, c0:c0 + cn], pb[:, 0:cn], g[:, 0:1], rbuf[:, k, c0:c0 + cn], ALU.mult, ALU.add)
            else:
                t3 = tmpB.rearrange("p (j s) -> p j s", j=8)
                TT(t3, pb[:, 0:128].rearrange("p (j s) -> p j s", j=8), g[:, 1:17].unsqueeze(1).to_broadcast([128, 8, 16]), ALU.mult)
                TT(rbuf[:, k, c0:c0 + cn], rbuf[:, k, c0:c0 + cn], tmpB, ALU.add)

        def ln_affine_inplace(L, goff):
            m = L["mean"][:, 0:NTc]; r_ = L["rstd"][:, 0:NTc]
            for k in range(KQ):
                x_ = rbuf[:, k, 0:NTc]
                (TTP if k % 2 == 0 else TT)(x_, x_, m, ALU.subtract)
                TT(x_, x_, r_, ALU.mult)
                ACT(x_, x_, AF.Identity, scale=gA[:, goff + k:goff + k + 1], bias=gA[:, goff + 16 + k:goff + 17 + k])

        def ada_bias_load(la):
            stb = f32v(alloc(128), 128)
            DMA(stb[0:96, :], b_ada[la].rearrange("(c p) -> c p", p=128))
            pb_ = nb(); TR(pb_[:, 0:96], stb[0:96, :], ident[0:96, 0:96]); CP(badaN, pb_[:, 0:96])
            cur[0] -= 128

        def ada_block(la, blk):
            wv_ = wget([wpart(w_ada[la], 0, KQ, blk * 512, 512)]).rearrange("p (k n) -> p k n", k=KQ)
            stg_ = astage[blk % 2].rearrange("p (c b) -> p c b", c=4)
            for m in range(4):
                ch = blk * 4 + m
                pb_ = nb()
                for k in range(KQ):
                    MM(pb_[:, 0:17], wv_[:, k, m * 128:(m + 1) * 128], scT[:, k, :], k == 0, k == KQ - 1)
                if ch // 16 in (1, 4):
                    TS(stg_[:, m, :], pb_[:, 0:17], badaN[:, ch:ch + 1], 1.0, ALU.add, ALU.add)
                else:
                    TS(stg_[:, m, :], pb_[:, 0:17], badaN[:, ch:ch + 1], None, ALU.add)
            DMA(ada_scr[la][:, blk * 68:(blk + 1) * 68], astage[blk % 2], w=[("ada_scr", la)])

        phase0 = cur[0]
        if isA:
            ada_bias_load(0)
            for blk in range(24):
                ada_block(0, blk)
        for l in range(depth):
            cur[0] = phase0
            last = (l == DEPTH - 1)
            o_st = alloc(128); st = f32v(o_st, 128)
            pvw = lambda v: v.rearrange("(c p) -> c p", p=128)
            DMA(st[0:96, :], pvw(b_ada[l])); DMA(st[96:112, :], pvw(ln1_g_d[l])); DMA(st[112:128, :], pvw(ln1_b_d[l]))
            pb = nb(); TR(pb[:, 0:128], st, ident); CP(pv1, pb[:, 0:128])
            for i_, src in enumerate([ln2_g_d, ln2_b_d]):
                DMA(st[i_ * 16:(i_ + 1) * 16, :], pvw(src[l]))
            for i_, src in enumerate([conv_b_d, cln_g_d, cln_b_d, ssm_d_d]):
                DMA(st[32 + i_ * 8:32 + (i_ + 1) * 8, :], pvw(src[l]))
            pb = nb(); TR(pb[:, 0:64], st[0:64, :], ident[0:64, 0:64]); CP(pv2, pb[:, 0:64])
            cwv = conv_w_d[l].rearrange("w (q p) -> (w q) p", p=128)
            DMA(st[0:128, :], cwv[0:128, :])
            pb = nb(); TR(pb[:, 0:128], st, ident); CP(cw[:, 0:128], pb[:, 0:128])
            DMA(st[0:120, :], cwv[128:248, :])
            pb = nb(); TR(pb[:, 0:120], st[0:120, :], ident[0:120, 0:120]); CP(cw[:, 128:248], pb[:, 0:120])
            TS(gA[:, 0:32], pv1[:, 96:128], ALPHA, None, ALU.mult)
            TS(gA[:, 32:64], pv2[:, 0:32], (1.0 if last else ALPHA), None, ALU.mult)
            DMA(rbias[:, 0:4], mbg_d[l].partition_broadcast(128))
            DMA(rbias[:, 4:36], mbr_d[l].partition_broadcast(128))
            cur[0] = o_st

            adaflat = adaT.rearrange("p c b -> p (c b)")
            DMA(adaflat, ada_scr[l], r=[("ada_scr", l)])

            L = ln_alloc()
            ln_stats(L, lambda k: rbuf[:, k, 0:NTc], KQ, ALPHA * ALPHA * LN_EPS)
            modulate_to_h(L, 1, 0)
            for k in range(KQ):
                DMA(xscr[k][:, 0:NTc], rbuf[:, k, 0:NTc], w=[("xscr", k)])
            if l == 0: dbg("h1", hT[:, :, 0:NTc])
            cur[0] = o_r

            if isA:
                ssm_prep(l)
            DMA(A8, A8_scr[l], r=[("A8_scr", l)])
            A8r = A8[:, 0:32]; A8i = A8[:, 32:64]
            cur[0] = o_r

            ysT = bf16v(alloc(8 * NT // 2), 8 * NT).rearrange("p (q n) -> p q n", q=8)
            ph1 = cur[0]
            Ucm = bf16v(alloc(4096), 8192).rearrange("p (g j k) -> p g j k", g=64, j=8)
            Um = bf16v(alloc(64 * 144 // 2), 64 * 144).rearrange("p (g c) -> p g c", g=64)
            Vm = f32v(alloc(2 * 32 * 145), 2 * 32 * 145).rearrange("p (r g c) -> p r g c", r=2, g=32)
            NCH = 144 if isA else 128
            wslots = []
            for half in range(2):
                wv = wget([wpart(w_in[l], 0, KQ, half * 512, 512)]).rearrange("p (k n) -> p k n", k=KQ)
                wslots.append(wv)
                for j in range(8):
                    pb = nb()
                    for k in range(KQ):
                        MM(pb[:, 0:512], hT[:, k, j * 128:(j + 1) * 128], wv[:, k, :], k == 0, k == KQ - 1)
                    ACP(Ucm[:, half * 32:(half + 1) * 32, j, :], pb[:, 0:512].rearrange("p (g k) -> p g k", g=32))
                if isA:
                    pass
            for g4 in range(16):
                pb = nb()
                pbb = pb[:, 0:256].bitcast(BF16).rearrange("p (g c) -> p g c", g=4)
                for q in range(4):
                    TR(pbb[:, q, :], Ucm[:, g4 * 4 + q].rearrange("p j k -> p (j k)"), identb)
                ACP(Um[:, g4 * 4:(g4 + 1) * 4, 0:128], pbb)
            if isA:
                for half in range(2):
                    wv = wget([wpart(w_in[l], 0, KQ, half * 512, 512)]).rearrange("p (k n) -> p k n", k=KQ)
                    for j in range(8):
                        pb = nb()
                        for k in range(KQ):
                            MM(pb[0:16, 0:512], hT[:, k, NP + j * 16:NP + (j + 1) * 16], wv[:, k, :], k == 0, k == KQ - 1)
                        ACT(Ucm[0:16, half * 32:(half + 1) * 32, j, :], pb[0:16, 0:512].rearrange("p (g k) -> p g k", g=32), AF.Copy)
                for g4 in range(16):
                    pb = nb()
                    pbb = pb[:, 0:32].bitcast(BF16).rearrange("p (g c) -> p g c", g=4)
                    for q in range(4):
                        TR(pbb[:, q, :], Ucm[0:16, g4 * 4 + q].rearrange("p j k -> p (j k)"), identb[0:16, 0:16])
                    ACP(Um[:, g4 * 4:(g4 + 1) * 4, 128:144], pbb)
            Rw = wget([(lambda s: s, R_scr[l], [("R_scr", l)])]).rearrange("p (g r q) -> p g r q", g=64, r=2)
            for gg in range(32):
                pb = nb()
                for ri_ in range(2):
                    for gh in range(2):
                        g = gh * 32 + gg
                        MM(pb[gh * 64:(gh + 1) * 64, ri_ * NCH:(ri_ + 1) * NCH], Rw[:, g, ri_, :], Um[:, g, 0:NCH], True, True)
                ACP(Vm[:, :, gg, 1:1 + NCH], pb[:, 0:2 * NCH].rearrange("p (r c) -> p r c", r=2))
            if isA:
                MS(Vm[:, :, :, 0], 0.0)
            else:
                CP(Vm[:, :, :, 0], stsave[:, l, :, :])
            stgA = f32v(alloc(128), 128); stgB = f32v(alloc(128), 128); ytmp = f32v(alloc(512), 512)
            s_t1 = f32v(alloc(64), 64).rearrange("p (r g) -> p r g", r=2)
            s_t2 = f32v(alloc(64), 64).rearrange("p (r g) -> p r g", r=2)
            A8rb = A8r.unsqueeze(1).to_broadcast([128, 2, 32])

            def cstep(prev, curc):
                TT(s_t1, prev, A8rb, ALU.mult)
                TT(s_t2[:, 0, :], prev[:, 1, :], A8i, ALU.mult)
                TT(s_t2[:, 1, :], prev[:, 0, :], A8i, ALU.mult)
                TT(s_t1, s_t1, curc, ALU.add)
                TT(curc[:, 0, :], s_t1[:, 0, :], s_t2[:, 0, :], ALU.subtract)
                TT(curc[:, 1, :], s_t1[:, 1, :], s_t2[:, 1, :], ALU.add)
            for c in range(128):
                cstep(Vm[:, :, :, c], Vm[:, :, :, c + 1])
            if isA:
                CP(stsave[:, l, :, :], Vm[:, :, :, 128])
            if (not isA) or ("B" not in passes):
                for ri_, od in enumerate([pre_o, pim_o]):
                    pb = nb()
                    TR(pb[0:32, 0:128], Vm[:, ri_, :, 128], ident)
                    CP(stgA[0:32, :], pb[0:32, 0:128])
                    DMA(od[l].rearrange("(h g) p -> g h p", h=2), stgA[0:32, :].rearrange("g (h p) -> g h p", h=2))
            if isA:
                Sis = f32v(alloc(1024), 1024).rearrange("p (r g s) -> p r g s", r=2, g=32)
                for ri_, sd in enumerate([sre_i, sim_i]):
                    for s4 in range(4):
                        sst = stgA
                        for gh in range(2):
                            for s1 in range(4):
                                DMA(sst[s1 * 32:(s1 + 1) * 32, gh * 64:(gh + 1) * 64], sd[l][s4 * 4 + s1, gh * 32:(gh + 1) * 32, :])
                        pb = nb()
                        TR(pb[:, 0:128], sst, ident)
                        CP(Sis[:, ri_, :, s4 * 4:(s4 + 1) * 4], pb[:, 0:128].rearrange("p (s g) -> p g s", s=4))
                s16a = f32v(alloc(1024), 1024).rearrange("p (r g s) -> p r g s", r=2, g=32)
                s16t = ytmp.rearrange("p (g s) -> p g s", g=32)
                b16 = lambda a: a.unsqueeze(2).to_broadcast([128, 32, 16])
                vs = Vm[:, :, :, 129:145]
                TT(s16a[:, 0], Sis[:, 0], b16(A8r), ALU.mult)
                TT(s16t, Sis[:, 1], b16(A8i), ALU.mult)
                TT(s16a[:, 0], s16a[:, 0], s16t, ALU.subtract)
                TT(s16a[:, 1], Sis[:, 1], b16(A8r), ALU.mult)
                TT(s16t, Sis[:, 0], b16(A8i), ALU.mult)
                TT(s16a[:, 1], s16a[:, 1], s16t, ALU.add)
                TT(vs, vs, s16a, ALU.add)
                for ri_, od in enumerate([sre_o, sim_o]):
                    for s4 in range(4):
                        pb = nb()
                        stg = stgA; stg2 = stgB
                        CP(stg.rearrange("p (s g) -> p s g", s=4), Vm[:, ri_, :, 129 + s4 * 4:133 + s4 * 4].rearrange("p g s -> p s g"))
                        TR(pb[:, 0:128], stg, ident)
                        CP(stg2, pb[:, 0:128])
                        for gh in range(2):
                            for s1 in range(4):
                                DMA(od[l][s4 * 4 + s1, gh * 32:(gh + 1) * 32, :], stg2[s1 * 32:(s1 + 1) * 32, gh * 64:(gh + 1) * 64])
            Tw = wget([(lambda s: s, T_scr[l], [("T_scr", l)])]).rearrange("p (g n) -> p g n", g=64)
            Ow = [wget([(lambda s: s.bitcast(F32), O_scr[l][ri_], [("O_scr", l)])], keep=True).bitcast(F32).rearrange("p (g n) -> p g n", g=32) for ri_ in range(2)]
            Vf = Vm
            Ycm = Ucm.rearrange("p g j k -> p (g j k)").rearrange("p (i n) -> p i n", i=8)
            for g4 in range(16):
                pbT = nb(); pbO = nb()
                for q in range(4):
                    g = g4 * 4 + q; gh = g // 32; gg = g % 32
                    sl = slice(gh * 64, (gh + 1) * 64)
                    MM(pbT[:, q * 128:(q + 1) * 128], Um[:, g, 0:128], Tw[:, g, :], True, True)
                    MM(pbO[:, q * 128:(q + 1) * 128], Vf[sl, 0, gg, 0:128], Ow[0][sl, gg, :], True, False)
                    MM(pbO[:, q * 128:(q + 1) * 128], Vf[sl, 1, gg, 0:128], Ow[1][sl, gg, :], False, True)
                ACP(ytmp, pbT[:, 0:512])
                TT(Ycm[:, :, g4 * 64:(g4 + 1) * 64].rearrange("p i (g k) -> p g i k", g=4), ytmp.rearrange("p (g i k) -> p g i k", g=4, i=8),
                   pbO[:, 0:512].rearrange("p (g i k) -> p g i k", g=4, i=8), ALU.add)
            for q in range(8):
                pb = nb()
                pbb = pb[:, 0:512].bitcast(BF16).rearrange("p (i c) -> p i c", i=8)
                for i in range(8):
                    TR(pbb[:, i, :], Ycm[:, i, q * 128:(q + 1) * 128], identb)
                ACT(ysT[:, q, 0:NP], pb[:, 0:512].bitcast(BF16), AF.Gelu_apprx_tanh)
            if isA:
                for g4 in range(16):
                    pbT = nb(); pbO = nb()
                    for q in range(4):
                        g = g4 * 4 + q; gh = g // 32; gg = g % 32
                        sl = slice(gh * 64, (gh + 1) * 64)
                        MM(pbT[0:16, q * 128:(q + 1) * 128], Um[:, g, 128:144], Tw[:, g, :], True, True)
                        MM(pbO[0:16, q * 128:(q + 1) * 128], Sis[sl, 0, gg, :], Ow[0][sl, gg, :], True, False)
                        MM(pbO[0:16, q * 128:(q + 1) * 128], Sis[sl, 1, gg, :], Ow[1][sl, gg, :], False, True)
                    CP(ytmp[0:16, :], pbT[0:16, 0:512])
                    TT(Ycm[0:16, :, g4 * 64:(g4 + 1) * 64].rearrange("p i (g k) -> p g i k", g=4), ytmp[0:16, :].rearrange("p (g i k) -> p g i k", g=4, i=8),
                       pbO[0:16, 0:512].rearrange("p (g i k) -> p g i k", g=4, i=8), ALU.add)
                for q in range(8):
                    pb = nb()
                    pbb = pb[:, 0:64].bitcast(BF16).rearrange("p (i c) -> p i c", i=8)
                    for i in range(8):
                        TR(pbb[:, i, :], Ycm[0:16, i, q * 128:(q + 1) * 128], identb[0:16, 0:16])
                    ACT(ysT[:, q, NP:NT], pb[:, 0:64].bitcast(BF16), AF.Gelu_apprx_tanh)
            if l == 0: dbg("ysT", ysT[:, :, 0:NTc])
            if l == 0 and debug:
                dbg("Ycm", Ycm); dbg("Um", Um); dbg("Vm", Vm)
                for nm, scr in (("Tscr", T_scr[l]), ("Rscr", R_scr[l]), ("O0", O_scr[l][0]), ("O1", O_scr[l][1])):
                    if (pas + "_" + nm) in dbg_o:
                        DMA(dbg_o[pas + "_" + nm], scr, r=[("T_scr", l), ("O_scr", l), ("R_scr", l)])
            if stop_after == "ssm":
                return
            cur[0] = ph1

            vT = bf16v(alloc(8 * NT // 2), 8 * NT).rearrange("p (q n) -> p q n", q=8)
            ph2 = cur[0]
            vbuf = f32v(alloc(8 * NT), 8 * NT).rearrange("p (q n) -> p q n", q=8)
            ph3 = cur[0]
            hist = f32v(alloc(240), 240).rearrange("p (q t) -> p q t", q=8)
            ubpb_l = [bf16v(alloc(528), 1056)[:, 0:1054] for _ in range(2)]
            ubsf_l = [f32v(alloc(608), 608).rearrange("p (s t) -> p s t", s=16) for _ in range(2)]
            ubsb_l = [bf16v(alloc(304), 608).rearrange("p (s t) -> p s t", s=16) for _ in range(2)]
            tailf_l = [f32v(alloc(32), 32) for _ in range(2)]
            dg = [bf16v(alloc(64), 128) for _ in range(31)]
            sg_t = f32v(alloc(512), 512)
            if isA:
                MS(hist, 0.0)
            else:
                CP(hist, cvsave[:, l, :, :])
            for q in range(8):
                ubpb = ubpb_l[q % 2]; ubsf = ubsf_l[q % 2]; ubsb = ubsb_l[q % 2]; tailf = tailf_l[q % 2]
                wv = wget([wpart(w_in[l], 0, KQ, 1024 + q * 128, 128, 0, 0), wpart(w_in[l], 0, KQ, 2048 + q * 128, 128, 0, 128)]).rearrange("p (k n) -> p k n", k=KQ)
                if isA:
                    for s4 in range(4):
                        sst = f32v(alloc(128), 128)
                        DMA(sst[0:120, :], scv_i[l][s4 * 4:(s4 + 1) * 4, :, q * 128:(q + 1) * 128].rearrange("s t c -> (s t) c"))
                        pb = nb()
                        TR(pb[:, 0:120], sst[0:120, :], ident[0:120, 0:120])
                        CP(ubsf[:, s4 * 4:(s4 + 1) * 4, 0:30], pb[:, 0:120].rearrange("p (s t) -> p s t", s=4))
                        cur[0] -= 128
                CP(ubpb[:, 0:30], hist[:, q, :])
                for w_ in range(31):
                    TS(dg[w_], identb, cw[:, w_ * 8 + q:w_ * 8 + q + 1], None, ALU.mult)
                for ci, (c0, cn) in enumerate(CT):
                    pa = nb(); pbb_ = nb()
                    for k in range(KQ):
                        MM(pa[:, 0:cn], wv[:, k, 0:128], hT[:, k, c0:c0 + cn], k == 0, k == KQ - 1)
                    for k in range(KQ):
                        MM(pbb_[:, 0:cn], wv[:, k, 128:256], hT[:, k, c0:c0 + cn], k == 0, k == KQ - 1)
                    ACT(sg_t[:, 0:cn], pbb_[:, 0:cn], AF.Sigmoid)
                    if ci < 2:
                        pav = pa[:, 0:512].rearrange("p (j c) -> p j c", j=4); sgv = sg_t.rearrange("p (j c) -> p j c", j=4)
                        dst = ubpb[:, 30:1054].rearrange("p (c j) -> p j c", j=8)[:, ci * 4:(ci + 1) * 4, :]
                        TT(dst, pav, sgv, ALU.mult)
                        TT(tailf.rearrange("p (c j) -> p j c", j=8)[:, ci * 4:(ci + 1) * 4, :], pav[:, :, 124:128], sgv[:, :, 124:128], ALU.mult)
                    else:
                        dst = ubsf[:, :, 30:38].rearrange("p s j -> p j s")
                        TT(dst, pa[:, 0:128].rearrange("p (j s) -> p j s", j=8), sg_t[:, 0:128].rearrange("p (j s) -> p j s", j=8), ALU.mult)
                        CP(ubsb, ubsf)
                for tt in range(2):
                    pv = nb()
                    for w_ in range(31):
                        MM(pv[:, 0:512], dg[w_], ubpb[:, w_ + tt * 512:w_ + tt * 512 + 512], w_ == 0, w_ == 30)
                    ACT(vbuf[:, q, tt * 512:(tt + 1) * 512], pv[:, 0:512], AF.Identity, bias=pv2[:, 32 + q:33 + q])
                if isA:
                    pv = nb()
                    for w_ in range(31):
                        MM(pv[:, 0:128].rearrange("p (s j) -> p s j", s=16), dg[w_], ubsb[:, :, w_:w_ + 8], w_ == 0, w_ == 30)
                    ACT(vbuf[:, q, NP:NT], pv[:, 0:128], AF.Identity, bias=pv2[:, 32 + q:33 + q])
                    CP(cvsave[:, l, q, :], tailf[:, 2:32])
                    for s4 in range(4):
                        stg = f32v(alloc(128), 128)
                        CP(stg[:, 0:120].rearrange("p (s t) -> p s t", s=4), ubsf[:, s4 * 4:(s4 + 1) * 4, 8:38])
                        pb = nb()
                        TR(pb[0:120, 0:128], stg[:, 0:120], ident)
                        stg2 = f32v(alloc(128), 128)
                        CP(stg2[0:120, :], pb[0:120, 0:128])
                        DMA(scv_o[l][s4 * 4:(s4 + 1) * 4, :, q * 128:(q + 1) * 128].rearrange("s t c -> (s t) c"), stg2[0:120, :])
                        cur[0] -= 256
                if (not isA) or ("B" not in passes):
                    stg = f32v(alloc(128), 128)
                    pb = nb()
                    TR(pb[0:30, 0:128], tailf[:, 2:32], ident)
                    CP(stg[0:30, :], pb[0:30, 0:128])
                    DMA(pcv_o[l][:, q * 128:(q + 1) * 128], stg[0:30, :])
                    cur[0] -= 128
            cur[0] = ph3
            L = ln_alloc()
            ln_stats(L, lambda k: vbuf[:, k, 0:NTc], 8, LN_EPS)
            m = L["mean"][:, 0:NTc]; r_ = L["rstd"][:, 0:NTc]
            for q in range(8):
                x_ = vbuf[:, q, 0:NTc]
                TT(x_, x_, m, ALU.subtract)
                TT(x_, x_, r_, ALU.mult)
                ACT(x_, x_, AF.Identity, scale=pv2[:, 40 + q:41 + q], bias=pv2[:, 48 + q:49 + q])
                ACT(vT[:, q, 0:NP].rearrange("p (j c) -> p c j", j=8), vbuf[:, q, 0:NP].rearrange("p (c j) -> p c j", j=8), AF.Silu)
                if isA:
                    ACT(vT[:, q, NP:NT].rearrange("p (j s) -> p s j", j=8), vbuf[:, q, NP:NT].rearrange("p (s j) -> p s j", s=16), AF.Silu)
            if l == 0: dbg("vT", vT[:, :, 0:NTc])

            cur[0] = ph2
            mix = bf16v(alloc(KQ * NT // 2), KQ * NT).rearrange("p (k n) -> p k n", k=KQ)
            tAB = [(f32v(alloc(512), 512), f32v(alloc(512), 512)) for _ in range(2)]
            tcnt = [0]
            for mb in range(8):
                wv = wget([wpart(w_in[l], 0, KQ, 3072 + mb * 256, 256, 0, 0), wpart(w_val[l], 0, 8, mb * 256, 256, 0, 256),
                           wpart(w_gate[l], 0, 8, mb * 256, 256, 8, 256)]).rearrange("p (k n) -> p k n", k=KQ)
                for m_ in range(2):
                    mch = mb * 2 + m_
                    for ci, (c0, cn) in enumerate(CT):
                        p1 = nb(); p2 = nb(); p3 = nb()
                        tA, tB = tAB[tcnt[0] % 2]; tcnt[0] += 1
                        for k in range(8):
                            MM(p1[:, 0:cn], wv[:, k, 256 + m_ * 128:256 + (m_ + 1) * 128], ysT[:, k, c0:c0 + cn], k == 0, k == 7)
                        for k in range(8):
                            MM(p2[:, 0:cn], wv[:, 8 + k, 256 + m_ * 128:256 + (m_ + 1) * 128], ysT[:, k, c0:c0 + cn], k == 0, k == 7)
                        for k in range(KQ):
                            MM(p3[:, 0:cn], wv[:, k, m_ * 128:(m_ + 1) * 128], hT[:, k, c0:c0 + cn], k == 0, k == KQ - 1)
                        ACT(tA[:, 0:cn], p2[:, 0:cn], AF.Sigmoid)
                        ACT(tB[:, 0:cn], p3[:, 0:cn], AF.Sigmoid)
                        TT(tA[:, 0:cn], tA[:, 0:cn], p1[:, 0:cn], ALU.mult)
                        TT(mix[:, mch, c0:c0 + cn], tA[:, 0:cn], tB[:, 0:cn], ALU.mult)
            for mb in range(8):
                wv = wget([wpart(w_in[l], 0, KQ, 5120 + mb * 256, 256, 0, 0), wpart(w_pw[l], 0, 8, mb * 256, 256, 0, 256)]).rearrange("p (k n) -> p k n", k=KQ)
                for m_ in range(2):
                    mch = mb * 2 + m_
                    for ci, (c0, cn) in enumerate(CT):
                        p1 = nb(); p3 = nb()
                        tA, tB = tAB[tcnt[0] % 2]; tcnt[0] += 1
                        for k in range(8):
                            MM(p1[:, 0:cn], wv[:, k, 256 + m_ * 128:256 + (m_ + 1) * 128], vT[:, k, c0:c0 + cn], k == 0, k == 7)
                        for k in range(KQ):
                            MM(p3[:, 0:cn], wv[:, k, m_ * 128:(m_ + 1) * 128], hT[:, k, c0:c0 + cn], k == 0, k == KQ - 1)
                        ACT(tB[:, 0:cn], p3[:, 0:cn], AF.Sigmoid)
                        TT(tB[:, 0:cn], tB[:, 0:cn], p1[:, 0:cn], ALU.mult)
                        TT(mix[:, mch, c0:c0 + cn], mix[:, mch, c0:c0 + cn], tB[:, 0:cn], ALU.add)
            if l == 0: dbg("mix", mix[:, :, 0:NTc])
            for k in range(KQ):
                CP(hT[:, k, 0:NTc], mix[:, k, 0:NTc])
            mix = hT
            cur[0] = o_r + KQ * NT
            tmpB = f32v(alloc(128), 128)
            for k in range(KQ):
                DMA(rbuf[:, k, 0:NTc], xscr[k][:, 0:NTc], r=[("xscr", k)])
            for mb in range(4):
                wv = wget([wpart(w_out[l], 0, KQ, mb * 512, 512)]).rearrange("p (k n) -> p k n", k=KQ)
                for m_ in range(4):
                    mch = mb * 4 + m_
                    for ci, (c0, cn) in enumerate(CT):
                        pb = nb()
                        for k in range(KQ):
                            MM(pb[:, 0:cn], wv[:, k, m_ * 128:(m_ + 1) * 128], mix[:, k, c0:c0 + cn], k == 0, k == KQ - 1)
                        resid_add(mch, ci, pb, 2, tmpB)
            cur[0] = o_r + KQ * NT
            L = ln_alloc()
            ln_stats(L, lambda k: rbuf[:, k, 0:NTc], KQ, LN_EPS)
            ln_affine_inplace(L, 0)
            if l == 0: dbg("x1", rbuf[:, :, 0:NTc])
            ln_stats(L, lambda k: rbuf[:, k, 0:NTc], KQ, ALPHA * ALPHA * LN_EPS)
            modulate_to_h(L, 4, 3)
            cur[0] = o_r + KQ * NT

            NTILE = NTc // 128
            comb = f32v(alloc(9 * 32), 288).rearrange("p (t e) -> p t e", t=9)
            combT = f32v(alloc(NT), NT)
            eselc = f32v(alloc(128), 128)
            lg = f32v(alloc(36), 36); m8 = f32v(alloc(8), 8); sm = f32v(alloc(16), 16)
            em = f32v(alloc(32), 32); msk = f32v(alloc(32), 32); gmk = f32v(alloc(4), 4); ge = f32v(alloc(4), 4)
            wr = wget([(lambda s: s.rearrange("p (k n) -> p k n", k=KQ)[:, :, 0:4], mwg_d[l].rearrange("(k p) n -> p k n", p=128), ()),
                       (lambda s: s.rearrange("p (k n) -> p k n", k=KQ)[:, :, 4:36], mwr_d[l].rearrange("(k p) n -> p k n", p=128), ())]).rearrange("p (k n) -> p k n", k=KQ)
            for t in range(NTILE):
                pb = nb()
                for k in range(KQ):
                    MM(pb[:, 0:36], hT[:, k, t * 128:(t + 1) * 128], wr[:, k, 0:36], k == 0, k == KQ - 1)
                TT(lg, pb[:, 0:36], rbias, ALU.add)
                glog = lg[:, 0:4]; elog = lg[:, 4:36]
                P.op("dve", (lambda o_=sm[:, 0:1], i_=glog: nc.vector.reduce_max(out=o_, in_=i_, axis=mybir.AxisListType.X)), ins=[glog], outs=[sm[:, 0:1]])
                TS(gmk, glog, sm[:, 0:1], None, ALU.is_equal)
                TS(sm[:, 1:2], sm[:, 0:1], -1.0, None, ALU.mult)
                ACT(ge, glog, AF.Exp, bias=sm[:, 1:2])
                P.op("dve", (lambda o_=sm[:, 2:3], i_=ge: nc.vector.reduce_sum(out=o_, in_=i_, axis=mybir.AxisListType.X)), ins=[ge], outs=[sm[:, 2:3]])
                RCP(sm[:, 3:4], sm[:, 2:3])
                TS(gmk, gmk, 1e30, -1e30, ALU.mult, ALU.add)
                TT(em.rearrange("p (g e) -> p g e", g=4), elog.rearrange("p (g e) -> p g e", g=4), gmk.unsqueeze(2).to_broadcast([128, 4, 8]), ALU.add)
                P.op("dve", (lambda o_=m8, i_=em: nc.vector.max(out=o_, in_=i_)), ins=[em], outs=[m8])
                TT(sm[:, 4:5], m8[:, 1:2], m8[:, 0:1], ALU.subtract)
                ACT(sm[:, 5:6], sm[:, 4:5], AF.Exp)
                TS(sm[:, 6:7], sm[:, 5:6], 1.0, None, ALU.add)
                RCP(sm[:, 7:8], sm[:, 6:7])
                TT(sm[:, 8:9], sm[:, 7:8], sm[:, 5:6], ALU.mult)
                TT(sm[:, 7:8], sm[:, 7:8], sm[:, 3:4], ALU.mult)
                TT(sm[:, 8:9], sm[:, 8:9], sm[:, 3:4], ALU.mult)
                TS(msk, em, m8[:, 0:1], sm[:, 7:8], ALU.is_equal, ALU.mult)
                TS(comb[:, t, :], em, m8[:, 1:2], sm[:, 8:9], ALU.is_equal, ALU.mult)
                TT(comb[:, t, :], comb[:, t, :], msk, ALU.add)
                pb = nb()
                TR(pb[0:32, 0:128], comb[:, t, :], ident)
                CP(combT[0:32, t * 128:(t + 1) * 128], pb[0:32, 0:128])
            if l == 0: dbg("combT", combT[0:32, 0:NTc])
            EP = 4
            act = bf16v(alloc(EP * 2 * NT // 2), EP * 2 * NT).rearrange("p (a n) -> p a n", a=EP * 2)
            tA = f32v(alloc(512), 512); tB = f32v(alloc(512), 512); tmpB = f32v(alloc(128), 128)
            do_next_ada = isA and (l + 1 < depth)
            if do_next_ada:
                ada_bias_load(l + 1)
            for ep in range(32 // EP):
                if do_next_ada:
                    for blk in range(ep * 3, ep * 3 + 3):
                        ada_block(l + 1, blk)
                for el in range(EP):
                    e = ep * EP + el
                    wv = wget([(lambda s: s.rearrange("p (k n) -> p k n", k=KQ)[:, :, 0:256], w_up[l, e].rearrange("(k p) n -> p k n", p=128), ()),
                               (lambda s: s.rearrange("p (k n) -> p k n", k=KQ)[:, :, 256:512], w_gt[l, e].rearrange("(k p) n -> p k n", p=128), ())]).rearrange("p (k n) -> p k n", k=KQ)
                    CP(eselc[0:32, :], ident[0:32, e:e + 1].to_broadcast([32, 128]))
                    for fc in range(2):
                        for ci, (c0, cn) in enumerate(CT):
                            pu = nb(); pg = nb(); pc = nb()
                            for k in range(KQ):
                                MM(pu[:, 0:cn], wv[:, k, fc * 128:(fc + 1) * 128], hT[:, k, c0:c0 + cn], k == 0, k == KQ - 1)
                            for k in range(KQ):
                                MM(pg[:, 0:cn], wv[:, k, 256 + fc * 128:256 + (fc + 1) * 128], hT[:, k, c0:c0 + cn], k == 0, k == KQ - 1)
                            MM(pc[:, 0:cn], eselc[0:32, :], combT[0:32, c0:c0 + cn], True, True)
                            ACT(tA[:, 0:cn], pg[:, 0:cn], AF.Silu)
                            TT(tA[:, 0:cn], tA[:, 0:cn], pu[:, 0:cn], ALU.mult)
                            TT(act[:, el * 2 + fc, c0:c0 + cn], tA[:, 0:cn], pc[:, 0:cn], ALU.mult)
                for mb in range(4):
                    src = w_dn[l, ep * EP:(ep + 1) * EP, :, mb * 512:(mb + 1) * 512].rearrange("e (f p) n -> p (e f) n", p=128)
                    wv = wget([(lambda s: s.rearrange("p (k n) -> p k n", k=KQ)[:, 0:EP * 2, :], src, ())]).rearrange("p (k n) -> p k n", k=KQ)
                    for m_ in range(4):
                        mch = mb * 4 + m_
                        for ci, (c0, cn) in enumerate(CT):
                            pb = nb()
                            for a in range(EP * 2):
                                MM(pb[:, 0:cn], wv[:, a, m_ * 128:(m_ + 1) * 128], act[:, a, c0:c0 + cn], a == 0, a == EP * 2 - 1)
                            resid_add(mch, ci, pb, 5, tmpB)
            cur[0] = o_r + KQ * NT
            L = ln_alloc()
            ln_stats(L, lambda k: rbuf[:, k, 0:NTc], KQ, LN_EPS)
            ln_affine_inplace(L, 32)
            if l == 0: dbg("x2", rbuf[:, :, 0:NTc])
            cur[0] = o_r + KQ * NT

        cur[0] = o_r + KQ * NT
        yt = [f32v(alloc(D), D), f32v(alloc(D), D)]
        yseg = yp_o[seg * NP:(seg + 1) * NP, :].rearrange("(c j) d -> j c d", j=8)
        for j in range(9 if isA else 8):
            t = yt[j % 2]
            for g4 in range(4):
                pb = nb()
                for q in range(4):
                    kq = g4 * 4 + q
                    TR(pb[:, q * 128:(q + 1) * 128], rbuf[:, kq, j * 128:(j + 1) * 128], ident)
                CP(t[:, g4 * 512:(g4 + 1) * 512], pb[:, 0:512])
            if j < 8:
                DMA(yseg[j], t)
            else:
                for jj in range(8):
                    DMA(ys_o[:, jj, :], t[jj * 16:(jj + 1) * 16, :])
        cur[0] = p0

    def ssm_prep(l):
        c0_ = cur[0]
        o_apw = alloc(640)
        a8t = f32v(alloc(64), 64)
        c_l0 = cur[0]
        o_l0 = alloc(128 * 26)

        def L0(i): return f32v(o_l0 + i * 128, 128)[0:32, :]
        are, aim, mag, ang, t0, t1, cosv, sinv, kre, kim, den = [L0(i) for i in range(11)]
        pw_re = [L0(11 + i) for i in range(7)]; pw_im = [L0(18 + i) for i in range(7)]
        dtt = f32v(alloc(2), 2)[0:32, :]
        v3 = lambda a: a.rearrange("g (h p) -> g h p", h=2)
        DMA(v3(are), a_re_d[l].rearrange("(h g) p -> g h p", h=2))
        DMA(v3(aim), a_im_d[l].rearrange("(h g) p -> g h p", h=2))
        for h in range(2):
            DMA(dtt[:, h:h + 1], ldt_d[l][h * 32:(h + 1) * 32].rearrange("(g o) -> g o", o=1))
        ACT(dtt, dtt, AF.Exp)
        dtb = dtt.unsqueeze(2).to_broadcast([32, 2, 64])
        TT(v3(t0), v3(are), dtb, ALU.mult)
        ACT(mag, t0, AF.Exp)
        TT(v3(ang), v3(aim), dtb, ALU.mult)
        TWO_PI = 2.0 * math.pi; MAGIC = 12582912.0

        def sin_of(dst, shift):
            TS(t0, ang, shift, 1.0 / TWO_PI, ALU.add, ALU.mult)
            TS(t1, t0, MAGIC, None, ALU.add)
            TS(t1, t1, -MAGIC, None, ALU.add)
            TT(t0, t0, t1, ALU.subtract)
            ACT(dst, t0, AF.Sin, scale=TWO_PI)
        sin_of(sinv, 0.0)
        sin_of(cosv, math.pi / 2.0)
        ab_re, ab_im = cosv, sinv
        TT(ab_re, cosv, mag, ALU.mult)
        TT(ab_im, sinv, mag, ALU.mult)
        TT(den, are, are, ALU.mult)
        TT(t0, aim, aim, ALU.mult)
        TT(den, den, t0, ALU.add)
        RCP(den, den)
        TS(t1, ab_re, -1.0, None, ALU.add)
        TT(kre, t1, are, ALU.mult)
        TT(t0, ab_im, aim, ALU.mult)
        TT(kre, kre, t0, ALU.add)
        TT(kre, kre, den, ALU.mult)
        TT(kim, ab_im, are, ALU.mult)
        TT(t0, t1, aim, ALU.mult)
        TT(kim, kim, t0, ALU.subtract)
        TT(kim, kim, den, ALU.mult)
        prev_re, prev_im = ab_re, ab_im
        for i in range(7):
            nr, ni = pw_re[i], pw_im[i]
            TT(nr, prev_re, ab_re, ALU.mult)
            TT(t0, prev_im, ab_im, ALU.mult)
            TT(nr, nr, t0, ALU.subtract)
            TT(ni, prev_re, ab_im, ALU.mult)
            TT(t1, prev_im, ab_re, ALU.mult)
            TT(ni, ni, t1, ALU.add)
            prev_re, prev_im = nr, ni
        APW = f32v(o_apw, 576).rearrange("p (m r g) -> p m r g", m=9, r=2)
        KK = f32v(o_apw + 576, 64).rearrange("p (r g) -> p r g", r=2)
        MS(APW[:, 0, 0, :], 1.0); MS(APW[:, 0, 1, :], 0.0)
        srcs = [(ab_re, APW[:, 1, 0, :]), (ab_im, APW[:, 1, 1, :])]
        for i in range(7):
            srcs.append((pw_re[i], APW[:, i + 2, 0, :])); srcs.append((pw_im[i], APW[:, i + 2, 1, :]))
        srcs.append((kre, KK[:, 0, :])); srcs.append((kim, KK[:, 1, :]))
        for (s_ap, d_ap) in srcs:
            pb = nb()
            TR(pb[:, 0:32], s_ap, ident[0:32, 0:32])
            CP(d_ap, pb[:, 0:32])
        CP(a8t[:, 0:32], APW[:, 8, 0, :]); CP(a8t[:, 32:64], APW[:, 8, 1, :])
        DMA(A8_scr[l], a8t, w=[("A8_scr", l)])
        cur[0] = c_l0
        Bt = f32v(alloc(1024), 1024).rearrange("p (r g k) -> p r g k", r=2, g=32)
        for ri_, bd in enumerate([b_re_d, b_im_d]):
            for gh in range(2):
                DMA(Bt[gh * 64:(gh + 1) * 64, ri_, :, :], bd[l][gh * 32:(gh + 1) * 32].rearrange("g p k -> p g k"))
        Bb = f32v(alloc(1024), 1024).rearrange("p (r g k) -> p r g k", r=2, g=32)
        t5 = f32v(alloc(512), 512).rearrange("p (g k) -> p g k", g=32)
        t6 = f32v(alloc(512), 512).rearrange("p (g k) -> p g k", g=32)
        bc = lambda a: a.unsqueeze(2).to_broadcast([128, 32, 16])
        TT(Bb[:, 0], Bt[:, 0], bc(KK[:, 0, :]), ALU.mult)
        TT(t5, Bt[:, 1], bc(KK[:, 1, :]), ALU.mult)
        TT(Bb[:, 0], Bb[:, 0], t5, ALU.subtract)
        TT(Bb[:, 1], Bt[:, 1], bc(KK[:, 0, :]), ALU.mult)
        TT(t5, Bt[:, 0], bc(KK[:, 1, :]), ALU.mult)
        TT(Bb[:, 1], Bb[:, 1], t5, ALU.add)
        Er = f32v(alloc(8192), 8192).rearrange("p (r g j k) -> p r g j k", r=2, g=32, j=8)
        for j in range(8):
            m = 7 - j
            TT(Er[:, 0, :, j, :], Bb[:, 0], bc(APW[:, m, 0, :]), ALU.mult)
            TT(t5, Bb[:, 1], bc(APW[:, m, 1, :]), ALU.mult)
            TT(Er[:, 0, :, j, :], Er[:, 0, :, j, :], t5, ALU.subtract)
            TT(Er[:, 1, :, j, :], Bb[:, 1], bc(APW[:, m, 0, :]), ALU.mult)
            TT(t5, Bb[:, 0], bc(APW[:, m, 1, :]), ALU.mult)
            TT(Er[:, 1, :, j, :], Er[:, 1, :, j, :], t5, ALU.add)
        c1_ = cur[0]
        Rm = bf16v(alloc(4096), 8192).rearrange("p (g r q) -> p g r q", g=64, r=2)
        for gg in range(32):
            pb = nb()
            for ri_ in range(2):
                TR(pb[:, ri_ * 128:(ri_ + 1) * 128], Er[:, ri_, gg].rearrange("p j k -> p (j k)"), ident)
            for gh in range(2):
                CP(Rm[:, gh * 32 + gg, :, :], pb[:, 0:256].rearrange("p (r h q) -> p r h q", r=2, h=2)[:, :, gh, :])
        DMA(R_scr[l], Rm.rearrange("p g r q -> p (g r q)"), w=[("R_scr", l)])
        cur[0] = c1_
        CTt = f32v(alloc(1024), 1024).rearrange("p (r g k) -> p r g k", r=2, g=32)
        cst = f32v(alloc(128), 128)
        for ri_, cd in enumerate([c_re_d, c_im_d]):
            for g8 in range(4):
                for gh in range(2):
                    DMA(cst[:, gh * 64:(gh + 1) * 64], cd[l][gh * 32 + g8 * 8: gh * 32 + g8 * 8 + 8].rearrange("g k p -> (g k) p"))
                pb = nb()
                TR(pb[:, 0:128], cst, ident)
                CP(CTt[:, ri_, g8 * 8:(g8 + 1) * 8, :], pb[:, 0:128].rearrange("p (g k) -> p g k", g=8))
        nCi = f32v(alloc(512), 512).rearrange("p (g k) -> p g k", g=32)
        TS(nCi, CTt[:, 1], -1.0, None, ALU.mult)
        Kst = f32v(alloc(1024), 1024)
        for half in range(2):
            pb = nb()
            for gl in range(32):
                g = half * 32 + gl; gh = g // 32; gg = g % 32
                sl = slice(gh * 64, (gh + 1) * 64)
                MM(pb[:, gl * 16:(gl + 1) * 16], Er[sl, 0, gg].rearrange("p j k -> p (j k)"), CTt[sl, 0, gg, :], True, False)
                MM(pb[:, gl * 16:(gl + 1) * 16], Er[sl, 1, gg].rearrange("p j k -> p (j k)"), nCi[sl, gg, :], False, True)
            CP(Kst[:, half * 512:(half + 1) * 512], pb[:, 0:512])
        drep = f32v(alloc(64), 64)
        dsel = f32v(alloc(1024), 1024).rearrange("p (a m) -> p a m", a=8)
        for a in range(8):
            CP(dsel[:, a, :].rearrange("p (j k) -> p j k", j=8), ident[:, a * 16:(a + 1) * 16].unsqueeze(1).to_broadcast([128, 8, 16]))
        pb = nb()
        for a in range(8):
            MM(pb[:, a * 8:(a + 1) * 8], dsel[:, a, :], pv2[:, 56:64], True, True)
        CP(drep.rearrange("p (q a) -> p q a", q=8), pb[:, 0:64].rearrange("p (a q) -> p q a", a=8))
        Tm = bf16v(alloc(4096), 8192).rearrange("p (g n) -> p g n", g=64)
        Tf = f32v(alloc(4096), 4096).rearrange("p (g n) -> p g n", g=32)
        for half in range(2):
            for i in range(8):
                pb = nb()
                MM(pb[:, 0:512], shm[:, i, :], Kst[:, half * 512:(half + 1) * 512], True, True)
                CP(Tf[:, :, i * 16:(i + 1) * 16], pb[:, 0:512].rearrange("p (g k) -> p g k", g=32))
            for gl in range(32):
                g = half * 32 + gl
                STT(Tm[:, g, :], ident, drep[:, g:g + 1], Tf[:, gl, :], ALU.mult, ALU.add)
        DMA(T_scr[l], Tm.rearrange("p g n -> p (g n)"), w=[("T_scr", l)])
        Om = Tf.rearrange("p g n -> p (g n)").rearrange("p (g n) -> p g n", g=32)
        for ri_ in range(2):
            for i in range(8):
                ar_b = bc(APW[:, i + 1, 0, :]); ai_b = bc(APW[:, i + 1, 1, :])
                osl = Om[:, :, i * 16:(i + 1) * 16]
                if ri_ == 0:
                    TT(t5, CTt[:, 0], ar_b, ALU.mult)
                    TT(t6, CTt[:, 1], ai_b, ALU.mult)
                    TT(osl, t5, t6, ALU.subtract)
                else:
                    TT(t5, CTt[:, 0], ai_b, ALU.mult)
                    TT(t6, nCi, ar_b, ALU.mult)
                    TT(osl, t6, t5, ALU.subtract)
            DMA(O_scr[l][ri_], Om.rearrange("p g n -> p (g n)"), w=[("O_scr", l)])
        cur[0] = c0_

    P.dry = True
    body()
    P.dry = False
    P.ops = []
    wstate["i"] = 0; wstate["issued"] = 0; wstate["rec"] = False
    body()
    P.emit()
    return nc, es, P


_SHM = None


def _consts():
    global _SHM
    if _SHM is None:
        shm = np.zeros((8, 128, 128), np.float32)
        for i in range(8):
            for j in range(i + 1):
                s = 7 - i + j
                for k in range(16):
                    shm[i, s * 16 + k, j * 16 + k] = 1.0
        _SHM = shm
    return np.eye(128, dtype=np.float32), _SHM


_WNAMES = ["w_ada", "b_ada", "w_in", "ssm_a_re", "ssm_a_im", "ssm_log_dt", "ssm_b_re", "ssm_b_im", "ssm_c_re", "ssm_c_im",
           "ssm_d", "w_s5_val", "w_s5_gate", "conv_w", "conv_b", "conv_ln_g", "conv_ln_b", "w_conv_pw", "w_out",
           "ln1_g", "ln1_b", "moe_w_group", "moe_b_group", "moe_w_router", "moe_b_router",
           "moe_w_up", "moe_w_gate", "moe_w_down", "ln2_g", "ln2_b"]


def make_in_maps(inputs):
    ident, shm = _consts()
    f = lambda a: np.ascontiguousarray(np.asarray(a, dtype=np.float32))
    shared = {n: f(inputs[n]) for n in _WNAMES}
    shared["ident"] = ident; shared["shm"] = shm
    xp = f(inputs["x_prompt"]); xs = f(inputs["x_sample"]); cp = f(inputs["c_prompt"]); cs = f(inputs["c_sample"])
    sre = f(inputs["state_ssm_re"]); sim = f(inputs["state_ssm_im"]); scv = f(inputs["state_conv"])
    maps = []
    for c in range(NCORES):
        b = c % 4
        m = dict(shared)
        m["xp"] = xp[b]
        m["xs"] = np.ascontiguousarray(xs[16 * c:16 * c + 16])
        m["cvec"] = np.ascontiguousarray(np.concatenate([cp[b:b + 1], cs[16 * c:16 * c + 16]], axis=0))
        m["sre"] = np.ascontiguousarray(sre[:, 16 * c:16 * c + 16])
        m["sim"] = np.ascontiguousarray(sim[:, 16 * c:16 * c + 16])
        m["scv"] = np.ascontiguousarray(scv[:, 16 * c:16 * c + 16])
        maps.append(m)
    return maps


_PROG = None


def kernel(**inputs):
    global _PROG
    if _PROG is None:
        _PROG = build_program()
    nc = _PROG[0]
    maps = make_in_maps(inputs)
    res = run_bass_kernel_spmd(nc, maps, core_ids=list(range(NCORES)))
    R = res.results
    y_prompt = np.stack([R[b]["yp"] for b in range(4)], axis=0)
    y_sample = np.concatenate([R[c]["ys"] for c in range(NCORES)], axis=0)
    p_re = np.stack([R[b]["pre"] for b in range(4)], axis=1)
    p_im = np.stack([R[b]["pim"] for b in range(4)], axis=1)
    p_cv = np.stack([R[b]["pcv"] for b in range(4)], axis=1)
    s_re = np.concatenate([R[c]["sre_o"] for c in range(NCORES)], axis=1)
    s_im = np.concatenate([R[c]["sim_o"] for c in range(NCORES)], axis=1)
    s_cv = np.concatenate([R[c]["scv_o"] for c in range(NCORES)], axis=1)
    f = lambda a: np.ascontiguousarray(a, dtype=np.float32)
    return (f(y_prompt), f(y_sample), f(p_re), f(p_im), f(p_cv), f(s_re), f(s_im), f(s_cv))
```
